# Optimizing a Trainium2 kernel written in Bass

```python
import jax
import jax.numpy as jnp
from jax import lax
import numpy as np

D_MODEL = 1024
BATCH = 4
SEQ = 4096
DEPTH = 2

GRID_W = 64
CTX_LEN = 256

NA_HEADS = 8
NA_HEAD_DIM = 64
NA_WIDTH = NA_HEADS * NA_HEAD_DIM
NA_WIN_ROWS = 8
NA_WIN_COLS = 16
ROPE_BASE = 10000.0
SC_WIDTH = 512
SC_CONV = 3
LRU_WIDTH = 512
LRU_BLOCKS = 8
LRU_BLOCK_DIM = LRU_WIDTH // LRU_BLOCKS
LRU_CONV = 4
LRU_C = 8.0
N_BRANCHES = 3
SPLIT_SIZES = (NA_WIDTH, NA_WIDTH, NA_WIDTH, SC_WIDTH, SC_WIDTH, SC_WIDTH,
               LRU_WIDTH, LRU_WIDTH, N_BRANCHES * D_MODEL)
P_TOTAL = sum(SPLIT_SIZES)
N_EXPERTS = 32
TOP_K = 4
D_EXPERT = D_MODEL
SWIGLU_LIMIT = 7.0
SWIGLU_ALPHA = 1.702
MOE_BLOCK = 128
LN_EPS = 1e-5
MOD_SCALE = 0.5
DEEPNORM_ALPHA = (2 * DEPTH) ** 0.25
DEEPNORM_BETA = (8 * DEPTH) ** -0.25

kernel_name = "hybrid_na_shortconv_rglru_moe_diffusion"


def layer_norm(x, gain=None, bias=None):
    xf = x.astype(jnp.float32)
    mu = jnp.mean(xf, axis=-1, keepdims=True)
    var = jnp.mean(jnp.square(xf - mu), axis=-1, keepdims=True)
    y = (xf - mu) * lax.rsqrt(var + LN_EPS)
    if gain is not None:
        y = y * gain.astype(jnp.float32) + bias.astype(jnp.float32)
    return y.astype(x.dtype)


def modulate(x, shift, scale):
    return x * (1.0 + scale) + shift


def depthwise_conv(u, w, pad_left, pad_right):
    return lax.conv_general_dilated(
        u, w.astype(u.dtype)[:, None, :], window_strides=(1,),
        padding=[(pad_left, pad_right)], dimension_numbers=("NWC", "WIO", "NWC"),
        feature_group_count=u.shape[-1])


def rope_1d(x, pos):
    m = x.shape[-1] // 2
    inv_freq = ROPE_BASE ** (-jnp.arange(m, dtype=jnp.float32) / m)
    ang = pos.astype(jnp.float32)[:, None] * inv_freq[None, :]
    cos = jnp.cos(ang)[None, :, None, :]
    sin = jnp.sin(ang)[None, :, None, :]
    xf = x.astype(jnp.float32)
    x1, x2 = xf[..., :m], xf[..., m:]
    return jnp.concatenate([x1 * cos - x2 * sin, x1 * sin + x2 * cos], axis=-1).astype(x.dtype)


def axial_rope(x, row_pos, col_pos):
    half = x.shape[-1] // 2
    return jnp.concatenate([rope_1d(x[..., :half], row_pos), rope_1d(x[..., half:], col_pos)], axis=-1)


def neighbourhood_attention(q, k, v, k_ctx, v_ctx, rpb):
    bsz, seq, heads, dh = q.shape
    rows = seq // GRID_W
    kr = min(NA_WIN_ROWS, rows)
    kc = NA_WIN_COLS
    n_loc = kr * kc
    scale = dh ** -0.5
    q5 = q.reshape(bsz, rows, GRID_W, heads, dh)
    k5 = k.reshape(bsz, rows, GRID_W, heads, dh)
    v5 = v.reshape(bsz, rows, GRID_W, heads, dh)
    cols = jnp.arange(GRID_W)
    col_start = jnp.clip(cols - kc // 2, 0, GRID_W - kc)
    col_idx = col_start[:, None] + jnp.arange(kc)[None, :]
    dc_idx = col_idx - cols[:, None] + (NA_WIN_COLS - 1)

    def row_block(r):
        r0 = jnp.clip(r - kr // 2, 0, rows - kr)
        dr_idx = r0 + jnp.arange(kr) - r + (NA_WIN_ROWS - 1)
        bias = rpb[:, dr_idx[None, :, None], dc_idx[:, None, :]].reshape(heads, GRID_W, n_loc)
        q_r = lax.dynamic_index_in_dim(q5, r, axis=1, keepdims=False)
        k_band = lax.dynamic_slice_in_dim(k5, r0, kr, axis=1)
        v_band = lax.dynamic_slice_in_dim(v5, r0, kr, axis=1)
        k_loc = k_band[:, :, col_idx].transpose(0, 2, 1, 3, 4, 5).reshape(bsz, GRID_W, n_loc, heads, dh)
        v_loc = v_band[:, :, col_idx].transpose(0, 2, 1, 3, 4, 5).reshape(bsz, GRID_W, n_loc, heads, dh)
        s_loc = jnp.einsum("bwhd,bwnhd->bhwn", q_r, k_loc).astype(jnp.float32) * scale \
            + bias.astype(jnp.float32)[None]
        s_ctx = jnp.einsum("bwhd,bchd->bhwc", q_r, k_ctx).astype(jnp.float32) * scale
        p = jax.nn.softmax(jnp.concatenate([s_loc, s_ctx], axis=-1), axis=-1).astype(v.dtype)
        return (jnp.einsum("bhwn,bwnhd->bwhd", p[..., :n_loc], v_loc)
                + jnp.einsum("bhwc,bchd->bwhd", p[..., n_loc:], v_ctx))

    o = lax.map(row_block, jnp.arange(rows))
    return o.transpose(1, 0, 2, 3, 4).reshape(bsz, seq, heads * dh)


def context_attention(q, k, v):
    bsz, n, heads, dh = q.shape
    s = jnp.einsum("bqhd,bkhd->bhqk", q, k).astype(jnp.float32) * dh ** -0.5
    p = jax.nn.softmax(s, axis=-1).astype(v.dtype)
    return jnp.einsum("bhqk,bkhd->bqhd", p, v).reshape(bsz, n, heads * dh)


def short_conv(b_gate, c_gate, xs, w):
    return b_gate * depthwise_conv(c_gate * xs, w, 1, 1)


def rglru_coeffs(xm, lam, w_r, b_r, w_i, b_i):
    bsz, seq, ch = xm.shape
    xb = xm.reshape(bsz, seq, LRU_BLOCKS, LRU_BLOCK_DIM)
    r = jax.nn.sigmoid(jnp.einsum("blnd,nde->blne", xb, w_r).reshape(bsz, seq, ch) + b_r).astype(jnp.float32)
    i = jax.nn.sigmoid(jnp.einsum("blnd,nde->blne", xb, w_i).reshape(bsz, seq, ch) + b_i)
    log_a = -LRU_C * jax.nn.softplus(-lam.astype(jnp.float32)) * r
    a = jnp.exp(log_a)
    b = jnp.sqrt(-jnp.expm1(2.0 * log_a)) * (i * xm).astype(jnp.float32)
    return a, b


def linear_scan(a, b, h0, reverse):
    edge = -1 if reverse else 0
    b = b.at[:, edge].add(a[:, edge] * h0)

    def combine(earlier, later):
        a_e, b_e = earlier
        a_l, b_l = later
        return a_e * a_l, a_l * b_e + b_l

    return lax.associative_scan(combine, (a, b), reverse=reverse, axis=1)[1]


def rglru_bidirectional(xm, xm_ctx, lam, w_r, b_r, w_i, b_i):
    h_lat, h_ctx = [], []
    for d, reverse in ((0, False), (1, True)):
        a_c, b_c = rglru_coeffs(xm_ctx, lam[d], w_r[d], b_r[d], w_i[d], b_i[d])
        hc = linear_scan(a_c, b_c, jnp.zeros_like(b_c[:, 0]), reverse)
        state = hc[:, 0] if reverse else hc[:, -1]
        a_l, b_l = rglru_coeffs(xm, lam[d], w_r[d], b_r[d], w_i[d], b_i[d])
        h_lat.append(linear_scan(a_l, b_l, state, reverse))
        h_ctx.append(hc)
    return (h_lat[0] + h_lat[1]).astype(xm.dtype), (h_ctx[0] + h_ctx[1]).astype(xm.dtype)


def merge_branches(gate_logits, y_a, y_b, y_c):
    g = jax.nn.sigmoid(gate_logits.reshape(gate_logits.shape[:-1] + (N_BRANCHES, D_MODEL)))
    return g[..., 0, :] * y_a + g[..., 1, :] * y_b + g[..., 2, :] * y_c


def mixer_sublayer(u, u_ctx, with_ctx_out, w_in, b_in, rpb, w_pa, w_pc, w_pl, sc_w,
                   lru_cw, lru_cb, lam, w_r, b_r, w_i, b_i, w_o, b_o):
    bsz, seq, _ = u.shape
    points = [int(p) for p in np.cumsum(SPLIT_SIZES)[:-1]]
    q, k, v, sb, scg, sx, lx, lg, gl = jnp.split(u @ w_in + b_in, points, axis=-1)
    qc, kc, vc, sbc, scgc, sxc, lxc, lgc, glc = jnp.split(u_ctx @ w_in + b_in, points, axis=-1)

    def heads(t):
        return t.reshape(t.shape[0], t.shape[1], NA_HEADS, NA_HEAD_DIM)

    t = jnp.arange(seq)
    row_pos, col_pos = t // GRID_W, t % GRID_W
    y_a = neighbourhood_attention(axial_rope(heads(q), row_pos, col_pos),
                                  axial_rope(heads(k), row_pos, col_pos),
                                  heads(v), heads(kc), heads(vc), rpb) @ w_pa
    y_b = short_conv(sb, scg, sx, sc_w) @ w_pc
    xm = depthwise_conv(lx, lru_cw, LRU_CONV // 2, LRU_CONV - 1 - LRU_CONV // 2) + lru_cb
    xm_c = depthwise_conv(lxc, lru_cw, LRU_CONV // 2, LRU_CONV - 1 - LRU_CONV // 2) + lru_cb
    h_lat, h_ctx = rglru_bidirectional(xm, xm_c, lam, w_r, b_r, w_i, b_i)
    y_c = (jax.nn.gelu(lg) * h_lat) @ w_pl
    out = merge_branches(gl, y_a, y_b, y_c) @ w_o + b_o
    if not with_ctx_out:
        return out, None
    y_a_c = context_attention(heads(qc), heads(kc), heads(vc)) @ w_pa
    y_b_c = short_conv(sbc, scgc, sxc, sc_w) @ w_pc
    y_c_c = (jax.nn.gelu(lgc) * h_ctx) @ w_pl
    out_c = merge_branches(glc, y_a_c, y_b_c, y_c_c) @ w_o + b_o
    return out, out_c


def routed_experts(tok, router_w, router_b, w_gu, b_gu, w_dn, b_dn):
    n_tok, d = tok.shape
    logits = (tok @ router_w + router_b).astype(jnp.float32)
    top_logits, top_idx = lax.top_k(logits, TOP_K)
    top_p = jax.nn.softmax(top_logits, axis=-1)
    n_assign = n_tok * TOP_K
    flat_e = top_idx.reshape(-1)
    flat_tok = jnp.repeat(jnp.arange(n_tok, dtype=jnp.int32), TOP_K)
    flat_p = top_p.reshape(-1)
    order = jnp.argsort(flat_e)
    s_e, s_tok, s_p = flat_e[order], flat_tok[order], flat_p[order]
    counts = jnp.bincount(flat_e, length=N_EXPERTS)
    padded = (counts + MOE_BLOCK - 1) // MOE_BLOCK * MOE_BLOCK
    start = jnp.cumsum(counts) - counts
    pad_end = jnp.cumsum(padded)
    pad_start = pad_end - padded
    dest = pad_start[s_e] + jnp.arange(n_assign) - start[s_e]
    n_blocks = -(-n_assign // MOE_BLOCK) + N_EXPERTS
    n_slots = n_blocks * MOE_BLOCK
    slot_tok = jnp.zeros((n_slots,), jnp.int32).at[dest].set(s_tok)
    slot_p = jnp.zeros((n_slots,), tok.dtype).at[dest].set(s_p.astype(tok.dtype))
    block_e = jnp.minimum(jnp.searchsorted(pad_end, jnp.arange(n_blocks) * MOE_BLOCK, side="right"),
                          N_EXPERTS - 1)
    x_blocks = tok[slot_tok].reshape(n_blocks, MOE_BLOCK, d)

    def expert_block(args):
        xb, e = args
        gate, up = jnp.split(xb @ w_gu[e] + b_gu[e], 2, axis=-1)
        gate = jnp.minimum(gate, SWIGLU_LIMIT)
        up = jnp.clip(up, -SWIGLU_LIMIT, SWIGLU_LIMIT)
        hid = (up + 1.0) * gate * jax.nn.sigmoid(SWIGLU_ALPHA * gate)
        return hid @ w_dn[e] + b_dn[e]

    y_blocks = lax.map(expert_block, (x_blocks, block_e))
    return jnp.zeros_like(tok).at[slot_tok].add(y_blocks.reshape(n_slots, d) * slot_p[:, None])


def setup_inputs(seed: int = 0) -> dict:
    key = jax.random.key(seed)
    ks = iter(jax.random.split(key, 40))

    def nrm(shape, scale):
        return jax.random.normal(next(ks), shape, jnp.float32) * scale

    d, f = D_MODEL, D_EXPERT
    a_c = jax.random.uniform(next(ks), (DEPTH, 2, LRU_WIDTH), jnp.float32, minval=0.9, maxval=0.999)
    a_base = a_c ** (1.0 / LRU_C)
    return {
        "x": nrm((BATCH, SEQ, d), 1.0),
        "c": nrm((BATCH, d), 1.0),
        "ctx": nrm((BATCH, CTX_LEN, d), 1.0),
        "c_ctx": nrm((d,), 1.0),
        "w_mod": nrm((DEPTH, d, 6 * d), MOD_SCALE * d ** -0.5),
        "b_mod": nrm((DEPTH, 6 * d), 0.02),
        "w_in": nrm((DEPTH, d, P_TOTAL), d ** -0.5),
        "b_in": nrm((DEPTH, P_TOTAL), 0.02),
        "na_rpb": nrm((DEPTH, NA_HEADS, 2 * NA_WIN_ROWS - 1, 2 * NA_WIN_COLS - 1), 0.1),
        "w_proj_attn": nrm((DEPTH, NA_WIDTH, d), DEEPNORM_BETA * NA_WIDTH ** -0.5),
        "w_proj_conv": nrm((DEPTH, SC_WIDTH, d), DEEPNORM_BETA * SC_WIDTH ** -0.5),
        "w_proj_lru": nrm((DEPTH, LRU_WIDTH, d), DEEPNORM_BETA * LRU_WIDTH ** -0.5),
        "sc_conv_w": nrm((DEPTH, SC_CONV, SC_WIDTH), SC_CONV ** -0.5),
        "lru_conv_w": nrm((DEPTH, LRU_CONV, LRU_WIDTH), LRU_CONV ** -0.5),
        "lru_conv_b": nrm((DEPTH, LRU_WIDTH), 0.02),
        "lru_lambda": jnp.log(a_base) - jnp.log1p(-a_base),
        "lru_w_r": nrm((DEPTH, 2, LRU_BLOCKS, LRU_BLOCK_DIM, LRU_BLOCK_DIM), LRU_BLOCK_DIM ** -0.5),
        "lru_b_r": nrm((DEPTH, 2, LRU_WIDTH), 0.02),
        "lru_w_i": nrm((DEPTH, 2, LRU_BLOCKS, LRU_BLOCK_DIM, LRU_BLOCK_DIM), LRU_BLOCK_DIM ** -0.5),
        "lru_b_i": nrm((DEPTH, 2, LRU_WIDTH), 0.02),
        "w_o": nrm((DEPTH, d, d), DEEPNORM_BETA * d ** -0.5),
        "b_o": nrm((DEPTH, d), 0.02),
        "ln1_g": 1.0 + nrm((DEPTH, d), 0.02),
        "ln1_b": nrm((DEPTH, d), 0.02),
        "router_w": nrm((DEPTH, d, N_EXPERTS), d ** -0.5),
        "router_b": nrm((DEPTH, N_EXPERTS), 0.01),
        "exp_w_gu": nrm((DEPTH, N_EXPERTS, d, 2 * f), d ** -0.5),
        "exp_b_gu": nrm((DEPTH, N_EXPERTS, 2 * f), 0.02),
        "exp_w_dn": nrm((DEPTH, N_EXPERTS, f, d), DEEPNORM_BETA * f ** -0.5),
        "exp_b_dn": nrm((DEPTH, N_EXPERTS, d), 0.02),
        "ln2_g": 1.0 + nrm((DEPTH, d), 0.02),
        "ln2_b": nrm((DEPTH, d), 0.02),
    }


def reference(x, c, ctx, c_ctx, w_mod, b_mod, w_in, b_in, na_rpb, w_proj_attn, w_proj_conv,
              w_proj_lru, sc_conv_w, lru_conv_w, lru_conv_b, lru_lambda, lru_w_r, lru_b_r,
              lru_w_i, lru_b_i, w_o, b_o, ln1_g, ln1_b, router_w, router_b, exp_w_gu,
              exp_b_gu, exp_w_dn, exp_b_dn, ln2_g, ln2_b):
    bsz, seq, d = x.shape
    n_ctx = ctx.shape[1]
    h, h_c = x, ctx
    for layer in range(DEPTH):
        last = layer == DEPTH - 1
        mod = jax.nn.silu(c) @ w_mod[layer] + b_mod[layer]
        mod_c = jax.nn.silu(c_ctx) @ w_mod[layer] + b_mod[layer]
        sh1, sc1, g1, sh2, sc2, g2 = jnp.split(mod[:, None, :], 6, axis=-1)
        csh1, csc1, cg1, csh2, csc2, cg2 = jnp.split(mod_c, 6, axis=-1)
        y, y_c = mixer_sublayer(
            modulate(layer_norm(h), sh1, sc1), modulate(layer_norm(h_c), csh1, csc1), not last,
            w_in[layer], b_in[layer], na_rpb[layer], w_proj_attn[layer], w_proj_conv[layer],
            w_proj_lru[layer], sc_conv_w[layer], lru_conv_w[layer], lru_conv_b[layer],
            lru_lambda[layer], lru_w_r[layer], lru_b_r[layer], lru_w_i[layer], lru_b_i[layer],
            w_o[layer], b_o[layer])
        h = layer_norm(DEEPNORM_ALPHA * h + g1 * y, ln1_g[layer], ln1_b[layer])
        u2 = modulate(layer_norm(h), sh2, sc2).reshape(bsz * seq, d)
        if last:
            y2 = routed_experts(u2, router_w[layer], router_b[layer], exp_w_gu[layer],
                                exp_b_gu[layer], exp_w_dn[layer], exp_b_dn[layer]).reshape(bsz, seq, d)
        else:
            h_c = layer_norm(DEEPNORM_ALPHA * h_c + cg1 * y_c, ln1_g[layer], ln1_b[layer])
            u2c = modulate(layer_norm(h_c), csh2, csc2).reshape(bsz * n_ctx, d)
            y2_all = routed_experts(jnp.concatenate([u2, u2c], axis=0), router_w[layer], router_b[layer],
                                    exp_w_gu[layer], exp_b_gu[layer], exp_w_dn[layer], exp_b_dn[layer])
            y2 = y2_all[:bsz * seq].reshape(bsz, seq, d)
            h_c = layer_norm(DEEPNORM_ALPHA * h_c + cg2 * y2_all[bsz * seq:].reshape(bsz, n_ctx, d),
                             ln2_g[layer], ln2_b[layer])
        h = layer_norm(DEEPNORM_ALPHA * h + g2 * y2, ln2_g[layer], ln2_b[layer])
    return h
```

```python
import numpy as np
from contextlib import ExitStack
import concourse.bass as bass
import concourse.mybir as mybir
from concourse.bass_utils import run_bass_kernel_spmd

F32 = mybir.dt.float32
BF16 = mybir.dt.bfloat16
ALU = mybir.AluOpType
AF = mybir.ActivationFunctionType
AX = mybir.AxisListType

L = 2
D = 1024
NCTX = 256
SEQ = 4096
T = NCTX + SEQ
GW = 64
NH = 8
NE = 32
PT_TOT = 7168
ALPHA = (2 * L) ** 0.25
EPS = 1e-5
NEG = -30000.0
TILES = [(0, 256)] + [(256 + 512 * i, 512) for i in range(8)]
NCORES = 4

_off = {}
_n = 0
for _name, _w in [("b_mod", 48), ("b_in", 56), ("b_qk", 16), ("b_v", 8), ("scw", 12), ("lcw", 16), ("lcb", 4),
                  ("lam", 8), ("lbr", 8), ("lbi", 8), ("b_o", 8), ("ln1g", 8), ("ln1b", 8), ("ln2g", 8),
                  ("ln2b", 8), ("rb", 32), ("bgu", 512)]:
    _off[_name] = (_n, _w)
    _n += _w
NS = _n


class FW:
    def __init__(self, nc, es, n_dma_sems=32):
        self.nc = nc
        self.eng = {"pe": nc.tensor, "dve": nc.vector, "act": nc.scalar, "pool": nc.gpsimd, "sp": nc.sync}
        self.sem = {k: es.enter_context(nc.semaphore("s_" + k)) for k in self.eng}
        self.cnt = {k: 0 for k in self.eng}
        self.seen = {k: {} for k in self.eng}
        self.dsem = [es.enter_context(nc.semaphore("d%d" % i)) for i in range(n_dma_sems)]
        self.dcnt = [0] * n_dma_sems
        self.dnext = 0
        self.lastw = {}
        self.readers = {}
        self.ninst = 0

    def _wait(self, e, tok):
        sem, val = tok
        key = id(sem)
        if self.seen[e].get(key, 0) >= val:
            return
        self.seen[e][key] = val
        self.eng[e].wait_ge(sem, val)

    def _deps(self, e, reads, writes):
        toks = []
        for b in reads:
            if b in self.lastw:
                toks.append(self.lastw[b])
        for b in writes:
            if b in self.lastw:
                toks.append(self.lastw[b])
            toks.extend(self.readers.get(b, ()))
        for t in toks:
            if e == "pe" and t[0] is self.sem["pe"]:
                continue
            self._wait(e, t)

    def _commit(self, tok, reads, writes):
        for b in reads:
            r = self.readers.setdefault(b, [])
            if len(r) > 12:
                best = {}
                for s, v in r:
                    if id(s) not in best or best[id(s)][1] < v:
                        best[id(s)] = (s, v)
                r[:] = list(best.values())
            r.append(tok)
        for b in writes:
            self.lastw[b] = tok
            self.readers[b] = []

    def op(self, e, fn, reads=(), writes=(), inc=True):
        self._deps(e, reads, writes)
        ins = fn(self.eng[e])
        tok = (self.sem[e], self.cnt[e] + 1)
        if inc:
            ins.then_inc(self.sem[e], 1)
            self.cnt[e] += 1
        self._commit(tok, reads, writes)
        self.ninst += 1
        return ins

    def dma(self, e, out, in_, reads=(), writes=(), **kw):
        k = self.dnext
        self.dnext = (self.dnext + 1) % len(self.dsem)
        if self.dcnt[k] > 0:
            self._wait(e, (self.dsem[k], self.dcnt[k]))
        self._deps(e, reads, writes)
        ins = self.eng[e].dma_start(out=out, in_=in_, **kw)
        self.dcnt[k] += 16
        ins.then_inc(self.dsem[k], 16)
        tok = (self.dsem[k], self.dcnt[k])
        self._commit(tok, reads, writes)
        self.ninst += 1
        return tok

    def barrier(self):
        for e in self.eng:
            for k in range(len(self.dsem)):
                if self.dcnt[k]:
                    self._wait(e, (self.dsem[k], self.dcnt[k]))
            for k in self.eng:
                if k != e and self.cnt[k]:
                    self._wait(e, (self.sem[k], self.cnt[k]))
        self.lastw = {}
        self.readers = {}


class Rot:
    def __init__(self, tiles, name):
        self.tiles = tiles
        self.name = name
        self.i = 0

    def next(self):
        k = self.i % len(self.tiles)
        self.i += 1
        return self.tiles[k], (self.name, k)


def build_program(stop_after=None, dbg=()):
    nc = bass.Bass("TRN2", target_bir_lowering=False)
    dbg = set(dbg)

    def din(name, shape, dt=F32):
        return nc.dram_tensor(name, list(shape), dt, kind="ExternalInput").ap()

    def dscr(name, shape, dt=F32):
        kind = "ExternalOutput" if name in dbg else "Internal"
        return nc.dram_tensor(name, list(shape), dt, kind=kind).ap()

    xT = din("xT", [128, 8, T])
    cvec = din("cvec", [128, 8, 2])
    w_mod = din("w_mod", [L, D, 6 * D])
    w_in = din("w_in", [L, D, PT_TOT])
    w_pa = din("w_pa", [L, 512, D])
    w_pc = din("w_pc", [L, 512, D])
    w_pl = din("w_pl", [L, 512, D])
    w_o = din("w_o", [L, D, D])
    wri = din("wri", [L, 2, 2, 4, 128, 128])
    router_w = din("router_w", [L, D, NE])
    w_gu = din("w_gu", [L, NE, D, 2 * D])
    w_dn = din("w_dn", [L, NE, D, D])
    b_dn = din("b_dn", [L, NE, D])
    small = din("small", [L, 128, NS])
    rpbx = din("rpbx", [L, NH, 128, 5, 640])
    amask = din("amask", [128, 5, 640])
    c_ident = din("c_ident", [128, 128])
    c_ones = din("c_ones", [128, 128])
    c_rot = din("c_rot", [64, 64])
    c_cos = din("c_cos", [64, SEQ])
    c_sin = din("c_sin", [64, SEQ])
    out = nc.dram_tensor("out", [128, 8, SEQ], F32, kind="ExternalOutput").ap()

    hT = dscr("hT", [128, 8, T])
    qT = dscr("qT", [NH, 64, T], BF16)
    kT = dscr("kT", [NH, 64, T], BF16)
    vtok = dscr("vtok", [128, 34, 512], BF16)
    secT = dscr("secT", [5, 128, 4, T])
    glT = dscr("glT", [128, 24, T])
    oT = dscr("oT", [NH, 64, T], BF16)
    cbT = dscr("cbT", [128, 4, T], BF16)
    lrT = dscr("lrT", [128, 4, T], BF16)
    u2T = dscr("u2T", [128, 8, T], BF16)
    pT_d = dscr("pT_d", [NE, T])

    with ExitStack() as es:
        fw = FW(nc, es)

        uid = [0]

        def sbt(st, name, shape, dt):
            uid[0] += 1
            return st.enter_context(nc.sbuf_tensor("%s_%d" % (name, uid[0]), list(shape), dt))

        def pst(st, name, shape, dt=F32):
            uid[0] += 1
            return st.enter_context(nc.psum_tensor("%s_%d" % (name, uid[0]), list(shape), dt))

        ident_f = sbt(es, "ident_f", [128, 128], F32)
        ident_b = sbt(es, "ident_b", [128, 128], BF16)
        ones_f = sbt(es, "ones_f", [128, 128], F32)
        rot_b = sbt(es, "rot_b", [64, 64], BF16)
        sm = sbt(es, "sm", [128, NS], F32)
        modT = sbt(es, "modT", [128, 48, 2], F32)
        pT = sbt(es, "pT", [NE, T], F32)
        cbias = sbt(es, "cbias", [128, 1], F32)
        fw.op("dve", lambda e: e.memset(cbias[:], 1.702 * 7.0), writes=["cbias"])
        fw.dma("sp", ident_f[:], c_ident, writes=["ident_f"])
        fw.dma("sp", ones_f[:], c_ones, writes=["ones_f"])
        fw.dma("pool", ident_b[:], c_ident, writes=["ident_b"])
        fw.dma("pool", rot_b[:], c_rot, writes=["rot_b"])

        def SM(name, i=None, n=1, rows=128):
            o, w = _off[name]
            if i is None:
                return sm[0:rows, o:o + w]
            return sm[0:rows, o + i:o + i + n]

        def ln_stats(st_pools, z, zkey, N):
            sq, sqk = st_pools["sq"].next()
            for kc in range(8):
                fw.op("act", lambda e, kc=kc: e.activation(sq[:, kc, 0:N], z[:, kc, 0:N], AF.Square),
                      reads=[zkey], writes=[sqk])
            pm, pmk = st_pools["ps"].next()
            pq, pqk = st_pools["ps"].next()
            for kc in range(8):
                fw.op("pe", lambda e, kc=kc: e.matmul(pm[:, 0:N], ones_f[:], z[:, kc, 0:N], start=(kc == 0), stop=(kc == 7)),
                      reads=[zkey, "ones_f"], writes=[pmk], inc=(kc == 7))
            for kc in range(8):
                fw.op("pe", lambda e, kc=kc: e.matmul(pq[:, 0:N], ones_f[:], sq[:, kc, 0:N], start=(kc == 0), stop=(kc == 7)),
                      reads=[sqk, "ones_f"], writes=[pqk], inc=(kc == 7))
            st, stk = st_pools["st"].next()
            fw.op("act", lambda e: e.activation(st[:, 0, 0:N], pm[:, 0:N], AF.Identity), reads=[pmk], writes=[stk])
            fw.op("dve", lambda e: e.tensor_tensor(st[:, 2, 0:N], st[:, 0, 0:N], st[:, 0, 0:N], ALU.mult), reads=[stk], writes=[stk])
            fw.op("dve", lambda e: e.tensor_tensor(st[:, 1, 0:N], pq[:, 0:N], st[:, 2, 0:N], ALU.subtract), reads=[stk, pqk], writes=[stk])
            fw.op("dve", lambda e: e.tensor_scalar(st[:, 1, 0:N], st[:, 1, 0:N], EPS, None, ALU.add), reads=[stk], writes=[stk])
            fw.op("act", lambda e: e.activation(st[:, 1, 0:N], st[:, 1, 0:N], AF.Sqrt), reads=[stk], writes=[stk])
            fw.op("dve", lambda e: e.reciprocal(st[:, 1, 0:N], st[:, 1, 0:N]), reads=[stk], writes=[stk])
            fw.op("dve", lambda e: e.tensor_tensor(st[:, 2, 0:N], st[:, 0, 0:N], st[:, 1, 0:N], ALU.mult), reads=[stk], writes=[stk])
            return st, stk

        def ln_apply(st, stk, z, zkey, N, outt, outkey, gain_fn, bias_fn, engs=("dve",)):
            for kc in range(8):
                e1 = engs[kc % len(engs)]
                fw.op(e1, lambda e, kc=kc: e.tensor_tensor(z[:, kc, 0:N], z[:, kc, 0:N], st[:, 1, 0:N], ALU.mult),
                      reads=[zkey, stk], writes=[zkey])
                fw.op(e1, lambda e, kc=kc: e.tensor_tensor(z[:, kc, 0:N], z[:, kc, 0:N], st[:, 2, 0:N], ALU.subtract),
                      reads=[zkey, stk], writes=[zkey])
                fw.op("act", lambda e, kc=kc: e.activation(outt[:, kc, 0:N], z[:, kc, 0:N], AF.Identity,
                                                           bias=bias_fn(kc), scale=gain_fn(kc)),
                      reads=[zkey, "modT", "sm"], writes=[outkey])

        fw.dma("sp", hT, xT, writes=["hT_all"])
        fw.barrier()

        for l in range(L):
            fw.dma("sp", sm[:], small[l], writes=["sm"])
            with ExitStack() as ph:
                csb = sbt(ph, "csb", [128, 8, 2], F32)
                cs2 = sbt(ph, "cs2", [128, 8, 2], F32)
                wm = Rot([sbt(ph, "wm%d" % i, [128, 8, 512], F32) for i in range(2)], "wm")
                pmod = pst(ph, "pmod", [128, 48, 2])
                fw.dma("sp", csb[:], cvec, writes=["csb"])
                fw.op("act", lambda e: e.activation(cs2[:], csb[:], AF.Silu), reads=["csb"], writes=["cs2"])
                for og in range(12):
                    wt, wk = wm.next()
                    fw.dma("sp", wt[:], w_mod[l][:, og * 512:(og + 1) * 512].rearrange("(kc p) n -> p kc n", p=128), writes=[wk])
                    for oc in range(4):
                        for kc in range(8):
                            fw.op("pe", lambda e, oc=oc, kc=kc: e.matmul(pmod[:, og * 4 + oc, :], wt[:, kc, oc * 128:(oc + 1) * 128],
                                                                         cs2[:, kc, :], start=(kc == 0), stop=(kc == 7)),
                                  reads=[wk, "cs2"], writes=["pmod"], inc=(kc == 7 and oc == 3))
                for j in range(2):
                    fw.op("dve", lambda e, j=j: e.tensor_tensor(modT[:, :, j], pmod[:, :, j], SM("b_mod"), ALU.add),
                          reads=["pmod", "sm"], writes=["modT"])
                for base in (8, 32):
                    fw.op("dve", lambda e, base=base: e.tensor_scalar_add(modT[:, base:base + 8, :], modT[:, base:base + 8, :], 1.0),
                          reads=["modT"], writes=["modT"])
                fw.barrier()
            if stop_after == "A":
                break

            def MOD(which, kc, ctx):
                j = 1 if ctx else 0
                return modT[:, which * 8 + kc, j:j + 1]

            with ExitStack() as ph:
                uT_sb = sbt(ph, "uT_sb", [128, 8, T], BF16)
                with ExitStack() as pb:
                    zp = Rot([sbt(pb, "zb%d" % i, [128, 8, 512], F32) for i in range(2)], "zb")
                    pools = {"sq": Rot([sbt(pb, "sqb%d" % i, [128, 8, 512], F32) for i in range(1)], "sqb"),
                             "st": Rot([sbt(pb, "stb%d" % i, [128, 3, 512], F32) for i in range(2)], "stb"),
                             "ps": Rot([pst(pb, "psb%d" % i, [128, 512]) for i in range(4)], "psb")}
                    for ti, (c0, N) in enumerate(TILES):
                        z, zk = zp.next()
                        fw.dma("sp", z[:, :, 0:N], hT[:, :, c0:c0 + N], reads=["hT_all"], writes=[zk])
                        st, stk = ln_stats(pools, z, zk, N)
                        ctx = (ti == 0)
                        ln_apply(st, stk, z, zk, N, uT_sb[:, :, c0:c0 + N], ("uT", ti),
                                 lambda kc: MOD(1, kc, ctx), lambda kc: MOD(0, kc, ctx), engs=("dve",))
                    fw.barrier()
                if stop_after == "B":
                    if "uT_dbg" in dbg:
                        pass
                    break
                with ExitStack() as pc:
                    wq = Rot([sbt(pc, "wq%d" % i, [128, 8, 512], BF16) for i in range(2)], "wq")
                    ev = Rot([sbt(pc, "ev%d" % i, [128, 4, 512], F32) for i in range(2)], "ev")
                    evb = Rot([sbt(pc, "evb%d" % i, [64, 512], BF16) for i in range(3)], "evb")
                    rt = Rot([sbt(pc, "rt%d" % i, [64, 2, 512], F32) for i in range(2)], "rt")
                    ob = Rot([sbt(pc, "ob%d" % i, [64, 512], BF16) for i in range(3)], "ob")
                    cos_t = sbt(pc, "cos_t", [64, SEQ], F32)
                    sin_t = sbt(pc, "sin_t", [64, SEQ], F32)
                    bq8 = sbt(pc, "bq8", [64, 8], F32)
                    vsb = sbt(pc, "vsb", [128, 34, 512], BF16)
                    pp = Rot([pst(pc, "pc%d" % i, [128, 512]) for i in range(6)], "pc")
                    fw.dma("sp", cos_t[:], c_cos, writes=["cos"])
                    fw.dma("sp", sin_t[:], c_sin, writes=["sin"])
                    fw.op("dve", lambda e: e.tensor_scalar(bq8[:], SM("b_qk", 0, 8, rows=64), 0.125, None, ALU.mult), reads=["sm"], writes=["bq8"])
                    uall = [("uT", ti) for ti in range(len(TILES))]
                    for sec in range(2):
                        wt, wk = wq.next()
                        fw.dma("pool", wt[:], w_in[l][:, sec * 512:(sec + 1) * 512].rearrange("(kc p) n -> p kc n", p=128), writes=[wk])
                        dst = qT if sec == 0 else kT
                        for h in range(NH):
                            for ti, (c0, N) in enumerate(TILES):
                                ps, pk = pp.next()
                                for kc in range(8):
                                    fw.op("pe", lambda e, kc=kc: e.matmul(ps[0:64, 0:N], wt[:, kc, h * 64:(h + 1) * 64], uT_sb[:, kc, c0:c0 + N],
                                                                          start=(kc == 0), stop=(kc == 7)),
                                          reads=[wk, ("uT", ti)], writes=[pk], inc=(kc == 7))
                                eb, ek = evb.next()
                                if sec == 0:
                                    fw.op("act", lambda e: e.activation(eb[:, 0:N], ps[0:64, 0:N], AF.Identity, bias=bq8[:, h:h + 1], scale=0.125),
                                          reads=[pk, "bq8"], writes=[ek])
                                else:
                                    fw.op("act", lambda e: e.activation(eb[:, 0:N], ps[0:64, 0:N], AF.Identity, bias=SM("b_qk", 8 + h, 1, rows=64)),
                                          reads=[pk, "sm"], writes=[ek])
                                if ti == 0:
                                    fw.dma("sp", dst[h][:, c0:c0 + N], eb[:, 0:N], reads=[ek], writes=[("qk", sec, h, ti)])
                                    continue
                                t0 = c0 - NCTX
                                pr, prk = pp.next()
                                fw.op("pe", lambda e: e.matmul(pr[0:64, 0:N], rot_b[:], eb[:, 0:N], start=True, stop=True),
                                      reads=[ek, "rot_b"], writes=[prk])
                                r2, rk = rt.next()
                                fw.op("dve", lambda e: e.tensor_tensor(r2[:, 0, 0:N], eb[:, 0:N], cos_t[:, t0:t0 + N], ALU.mult),
                                      reads=[ek, "cos"], writes=[rk])
                                fw.op("dve", lambda e: e.tensor_tensor(r2[:, 1, 0:N], pr[0:64, 0:N], sin_t[:, t0:t0 + N], ALU.mult),
                                      reads=[prk, "sin"], writes=[rk])
                                o2, ok = ob.next()
                                fw.op("dve", lambda e: e.tensor_tensor(o2[:, 0:N], r2[:, 0, 0:N], r2[:, 1, 0:N], ALU.add),
                                      reads=[rk], writes=[ok])
                                fw.dma("sp", dst[h][:, c0:c0 + N], o2[:, 0:N], reads=[ok], writes=[("qk", sec, h, ti)])
                    wt, wk = wq.next()
                    fw.dma("pool", wt[:], w_in[l][:, 1024:1536].rearrange("(kc p) n -> p kc n", p=128), writes=[wk])
                    for tc in range(34):
                        ps, pk = pp.next()
                        for kc in range(8):
                            fw.op("pe", lambda e, kc=kc: e.matmul(ps[:, :], uT_sb[:, kc, tc * 128:(tc + 1) * 128], wt[:, kc, :],
                                                                  start=(kc == 0), stop=(kc == 7)),
                                  reads=[wk] + uall, writes=[pk], inc=(kc == 7))
                        fw.op("act" if tc % 2 else "dve",
                              (lambda e: e.activation(vsb[:, tc, :], ps[:, :], AF.Identity)) if tc % 2 else (lambda e: e.tensor_copy(vsb[:, tc, :], ps[:, :])),
                              reads=[pk], writes=["vsb"])
                    fw.dma("sp", vtok, vsb[:], reads=["vsb"], writes=["vtok"])
                    for g in range(11):
                        col0 = 1536 + g * 512
                        wt, wk = wq.next()
                        fw.dma("pool", wt[:], w_in[l][:, col0:col0 + 512].rearrange("(kc p) n -> p kc n", p=128), writes=[wk])
                        for ti, (c0, N) in enumerate(TILES):
                            et, ek = ev.next()
                            for oc in range(4):
                                ps, pk = pp.next()
                                for kc in range(8):
                                    fw.op("pe", lambda e, kc=kc, oc=oc: e.matmul(ps[:, 0:N], wt[:, kc, oc * 128:(oc + 1) * 128], uT_sb[:, kc, c0:c0 + N],
                                                                                 start=(kc == 0), stop=(kc == 7)),
                                          reads=[wk, ("uT", ti)], writes=[pk], inc=(kc == 7))
                                bch = (col0 // 128) + oc
                                fw.op("act" if oc % 2 else "dve",
                                      (lambda e, oc=oc, bch=bch: e.activation(et[:, oc, 0:N], ps[:, 0:N], AF.Identity, bias=SM("b_in", bch))) if oc % 2 else
                                      (lambda e, oc=oc, bch=bch: e.tensor_scalar(et[:, oc, 0:N], ps[:, 0:N], SM("b_in", bch), None, ALU.add)),
                                      reads=[pk, "sm"], writes=[ek])
                            if g < 5:
                                fw.dma("sp", secT[g][:, :, c0:c0 + N], et[:, :, 0:N], reads=[ek], writes=[("sec", g, ti)])
                            else:
                                fw.dma("sp", glT[:, (g - 5) * 4:(g - 4) * 4, c0:c0 + N], et[:, :, 0:N], reads=[ek], writes=[("gl", g, ti)])
                    fw.barrier()
            if stop_after == "C":
                break

            with ExitStack() as ph:
                qh = Rot([sbt(ph, "qh%d" % i, [64, T], BF16) for i in range(2)], "qh")
                kh = Rot([sbt(ph, "kh%d" % i, [64, T], BF16) for i in range(2)], "kh")
                vh = Rot([sbt(ph, "vh%d" % i, [128, 34, 64], BF16) for i in range(2)], "vh")
                bf = Rot([sbt(ph, "bf%d" % i, [128, 5, 640], F32) for i in range(1)], "bf")
                mk = sbt(ph, "mk", [128, 5, 640], F32)
                bb = Rot([sbt(ph, "bb%d" % i, [128, 5, 640], BF16) for i in range(2)], "bb")
                oh = Rot([sbt(ph, "oh%d" % i, [64, T], BF16) for i in range(2)], "oh")
                pe_ = Rot([sbt(ph, "pe%d" % i, [128, 896], BF16) for i in range(4)], "pex")
                pn = Rot([sbt(ph, "pn%d" % i, [128, 896], BF16) for i in range(4)], "pn")
                pts = Rot([sbt(ph, "pts%d" % i, [128, 7, 128], BF16) for i in range(4)], "pts")
                stt = Rot([sbt(ph, "stt%d" % i, [128, 4], F32) for i in range(8)], "stt")
                pS = Rot([pst(ph, "pS%d" % i, [128, 1024]) for i in range(2)], "pS")
                pT_ = Rot([pst(ph, "pT%d" % i, [128, 7, 128], BF16) for i in range(2)], "pTT")
                pO = Rot([pst(ph, "pO%d" % i, [64, 128]) for i in range(2)], "pO")
                fw.dma("sp", mk[:], amask, writes=["mk"])

                heads = {}

                def load_head(h):
                    q_, qk_ = qh.next()
                    k_, kk_ = kh.next()
                    v_, vk_ = vh.next()
                    f_, fk_ = bf.next()
                    b_, bk_ = bb.next()
                    o_, ok_ = oh.next()
                    fw.dma("sp", q_[:], qT[h], writes=[qk_])
                    fw.dma("sp", k_[:], kT[h], writes=[kk_])
                    fw.dma("sp", v_[:], vtok[:, :, h * 64:(h + 1) * 64], writes=[vk_])
                    fw.dma("sp", f_[:], rpbx[l][h], writes=[fk_])
                    fw.op("dve", lambda e: e.tensor_tensor(b_[:], f_[:], mk[:], ALU.add), reads=[fk_, "mk"], writes=[bk_])
                    heads[h] = dict(q=q_, qk=qk_, k=k_, kk=kk_, v=v_, vk=vk_, b=b_, bk=bk_, o=o_, ok=ok_)

                its = []
                for h in range(NH):
                    for j in range(32):
                        bs = min(max(2 * j - 4, 0), 54)
                        its.append(dict(h=h, kind="na", qc0=NCTX + 128 * j, kc0=NCTX + 64 * bs, pat={0: 1, 1: 2, 30: 3, 31: 4}.get(j, 0),
                                        ncols=896, vch=[2 + bs // 2 + c for c in range(5)] + [0, 1], last=False))
                    for qc in range(2):
                        its.append(dict(h=h, kind="ctx", qc0=qc * 128, ncols=256, vch=[0, 1], last=(qc == 1)))

                def st0(it):
                    H = heads[it["h"]]
                    q_, k_, b_ = H["q"], H["k"], H["b"]
                    S, Sk = pS.next()
                    it["S"], it["Sk"] = S, Sk
                    qc0 = it["qc0"]
                    if it["kind"] == "na":
                        kc0, pat = it["kc0"], it["pat"]
                        fw.op("pe", lambda e: e.matmul(S[:, 0:512], q_[:, qc0:qc0 + 128], k_[:, kc0:kc0 + 512], start=True, stop=False),
                              reads=[H["qk"], H["kk"]], writes=[Sk], inc=False)
                        fw.op("pe", lambda e: e.matmul(S[:, 0:512], ident_b[:], b_[:, pat, 0:512], start=False, stop=True),
                              reads=[H["bk"], "ident_b"], writes=[Sk], inc=False)
                        fw.op("pe", lambda e: e.matmul(S[:, 512:640], q_[:, qc0:qc0 + 128], k_[:, kc0 + 512:kc0 + 640], start=True, stop=False),
                              reads=[H["qk"], H["kk"]], writes=[Sk], inc=False)
                        fw.op("pe", lambda e: e.matmul(S[:, 512:640], ident_b[:], b_[:, pat, 512:640], start=False, stop=True),
                              reads=[H["bk"], "ident_b"], writes=[Sk], inc=False)
                        fw.op("pe", lambda e: e.matmul(S[:, 640:896], q_[:, qc0:qc0 + 128], k_[:, 0:256], start=True, stop=True),
                              reads=[H["qk"], H["kk"]], writes=[Sk])
                    else:
                        fw.op("pe", lambda e: e.matmul(S[:, 0:256], q_[:, qc0:qc0 + 128], k_[:, 0:256], start=True, stop=True),
                              reads=[H["qk"], H["kk"]], writes=[Sk])

                def st1(it):
                    S, Sk, ncols = it["S"], it["Sk"], it["ncols"]
                    sst, ssk = stt.next()
                    fw.op("dve", lambda e: e.reduce_max(sst[:, 0:1], S[:, 0:ncols], AX.X), reads=[Sk], writes=[ssk])
                    fw.op("dve", lambda e: e.tensor_scalar(sst[:, 1:2], sst[:, 0:1], -1.0, None, ALU.mult), reads=[ssk], writes=[ssk])
                    px, pxk = pe_.next()
                    fw.op("act", lambda e: e.activation(px[:, 0:ncols], S[:, 0:ncols], AF.Exp, bias=sst[:, 1:2], accum_out=sst[:, 2:3]),
                          reads=[Sk, ssk], writes=[pxk, ssk])
                    it["sst"], it["ssk"], it["px"], it["pxk"] = sst, ssk, px, pxk

                def st1b(it):
                    sst, ssk, px, pxk, ncols = it["sst"], it["ssk"], it["px"], it["pxk"], it["ncols"]
                    fw.op("dve", lambda e: e.reciprocal(sst[:, 3:4], sst[:, 2:3]), reads=[ssk], writes=[ssk])
                    pnt, pnk = pn.next()
                    fw.op("dve", lambda e: e.tensor_scalar(pnt[:, 0:ncols], px[:, 0:ncols], sst[:, 3:4], None, ALU.mult),
                          reads=[pxk, ssk], writes=[pnk])
                    it["pn"], it["pnk"] = pnt, pnk

                def st2(it):
                    pnt, pnk, nch = it["pn"], it["pnk"], it["ncols"] // 128
                    ptp, ptk = pT_.next()
                    for c in range(nch):
                        fw.op("pe", lambda e, c=c: e.transpose(ptp[:, c, :], pnt[:, c * 128:(c + 1) * 128], ident_b[:]),
                              reads=[pnk, "ident_b"], writes=[ptk], inc=(c == nch - 1))
                    ptt, pttk = pts.next()
                    fw.op("act", lambda e: e.activation(ptt[:, 0:nch, :], ptp[:, 0:nch, :], AF.Identity), reads=[ptk], writes=[pttk])
                    it["pt"], it["ptk"] = ptt, pttk

                def st3(it):
                    H = heads[it["h"]]
                    v_, o_, h = H["v"], H["o"], it["h"]
                    ptt, pttk, nch, vch, qc0 = it["pt"], it["ptk"], it["ncols"] // 128, it["vch"], it["qc0"]
                    po, pok = pO.next()
                    for c in range(nch):
                        fw.op("pe", lambda e, c=c: e.matmul(po[:, :], v_[:, vch[c], :], ptt[:, c, :], start=(c == 0), stop=(c == nch - 1)),
                              reads=[H["vk"], pttk], writes=[pok], inc=(c == nch - 1))
                    fw.op("dve", lambda e: e.tensor_scalar(o_[:, qc0:qc0 + 128], po[:, :], SM("b_v", h, 1, rows=64), None, ALU.add),
                          reads=[pok, "sm"], writes=[H["ok"]])
                    if it["last"]:
                        fw.dma("sp", oT[h], o_[:], reads=[H["ok"]], writes=[("oT", h)])

                load_head(0)
                load_head(1)
                nit = len(its)
                for s in range(nit + 4):
                    if s < nit:
                        st0(its[s])
                    if 0 <= s - 1 < nit:
                        st1(its[s - 1])
                    if 0 <= s - 2 < nit:
                        st1b(its[s - 2])
                    if 0 <= s - 3 < nit:
                        st2(its[s - 3])
                    if 0 <= s - 4 < nit:
                        st3(its[s - 4])
                        hh_ = its[s - 4]["h"]
                        if its[s - 4]["last"] and hh_ + 2 < NH:
                            load_head(hh_ + 2)
                fw.barrier()
            if stop_after == "D":
                break

            SEGS = [(0, NCTX), (NCTX, T)]
            with ExitStack() as ph:
                a3 = [Rot([sbt(ph, "cv%d_%d" % (s, i), [128, T], F32) for i in range(2)], "cv%d" % s) for s in range(3)]
                acc = Rot([sbt(ph, "cacc%d" % i, [128, T], F32) for i in range(2)], "cacc")
                cbo = Rot([sbt(ph, "cbo%d" % i, [128, T], BF16) for i in range(2)], "cbo")
                for ch in range(4):
                    tl = []
                    for s in range(3):
                        t_, k_ = a3[s].next()
                        fw.dma("sp", t_[:], secT[s][:, ch, :], writes=[k_])
                        tl.append((t_, k_))
                    (sbt_, sbk), (cg, cgk), (sx, sxk) = tl
                    ac, ack = acc.next()
                    fw.op("dve", lambda e: e.tensor_tensor(cg[:], cg[:], sx[:], ALU.mult), reads=[cgk, sxk], writes=[cgk])
                    w = lambda k: SM("scw", ch * 3 + k)
                    fw.op("dve", lambda e: e.tensor_scalar(ac[:], cg[:], w(1), None, ALU.mult), reads=[cgk, "sm"], writes=[ack])
                    for (a, b) in SEGS:
                        fw.op("dve", lambda e, a=a, b=b: e.scalar_tensor_tensor(ac[:, a + 1:b], cg[:, a:b - 1], w(0), ac[:, a + 1:b], ALU.mult, ALU.add),
                              reads=[cgk, ack, "sm"], writes=[ack])
                        fw.op("dve", lambda e, a=a, b=b: e.scalar_tensor_tensor(ac[:, a:b - 1], cg[:, a + 1:b], w(2), ac[:, a:b - 1], ALU.mult, ALU.add),
                              reads=[cgk, ack, "sm"], writes=[ack])
                    co, cok = cbo.next()
                    fw.op("dve", lambda e: e.tensor_tensor(co[:], ac[:], sbt_[:], ALU.mult), reads=[ack, sbk], writes=[cok])
                    fw.dma("sp", cbT[:, ch, :], co[:], reads=[cok], writes=[("cbT", ch)])
                fw.barrier()
            if stop_after == "E":
                break

            with ExitStack() as ph:
                lx = sbt(ph, "lx", [128, T], F32)
                xm = sbt(ph, "xm", [128, T], F32)
                lg = sbt(ph, "lg", [128, T], F32)
                rr = sbt(ph, "rr", [128, T], F32)
                ii = sbt(ph, "ii", [128, T], F32)
                bbv = sbt(ph, "bbv", [128, T], F32)
                hf = [sbt(ph, "hf%d" % d, [128, T], F32) for d in range(2)]
                lro = sbt(ph, "lro", [128, T], BF16)
                wg = sbt(ph, "wg", [128, 2, 2, 128], F32)
                cc = sbt(ph, "cc", [128, 8], F32)
                pp = Rot([pst(ph, "pf%d" % i, [128, 512]) for i in range(4)], "pf")
                fw.op("act", lambda e: e.activation(cc[:], SM("lam"), AF.Exp, scale=-1.0), reads=["sm"], writes=["cc"])
                fw.op("act", lambda e: e.activation(cc[:], cc[:], AF.Ln, bias=1.0), reads=["cc"], writes=["cc"])
                fw.op("dve", lambda e: e.tensor_scalar(cc[:], cc[:], -8.0, None, ALU.mult), reads=["cc"], writes=["cc"])
                for ch in range(4):
                    fw.dma("sp", lx[:], secT[3][:, ch, :], writes=["lx"])
                    fw.dma("sp", lg[:], secT[4][:, ch, :], writes=["lg"])
                    fw.dma("sp", wg[:], wri[l][:, :, ch].rearrange("g d k m -> k g d m"), writes=["wg"])
                    w = lambda k: SM("lcw", ch * 4 + k)
                    fw.op("dve", lambda e: e.tensor_scalar(xm[:], lx[:], w(2), SM("lcb", ch), ALU.mult, ALU.add), reads=["lx", "sm"], writes=["xm"])
                    for (a, b) in SEGS:
                        fw.op("dve", lambda e, a=a, b=b: e.scalar_tensor_tensor(xm[:, a + 2:b], lx[:, a:b - 2], w(0), xm[:, a + 2:b], ALU.mult, ALU.add),
                              reads=["lx", "xm", "sm"], writes=["xm"])
                        fw.op("dve", lambda e, a=a, b=b: e.scalar_tensor_tensor(xm[:, a + 1:b], lx[:, a:b - 1], w(1), xm[:, a + 1:b], ALU.mult, ALU.add),
                              reads=["lx", "xm", "sm"], writes=["xm"])
                        fw.op("dve", lambda e, a=a, b=b: e.scalar_tensor_tensor(xm[:, a:b - 1], lx[:, a + 1:b], w(3), xm[:, a:b - 1], ALU.mult, ALU.add),
                              reads=["lx", "xm", "sm"], writes=["xm"])
                    for d in range(2):
                        for (c0, N) in TILES:
                            for g, dst in ((0, rr), (1, ii)):
                                ps, pk = pp.next()
                                fw.op("pe", lambda e, g=g: e.matmul(ps[:, 0:N], wg[:, g, d, :], xm[:, c0:c0 + N], start=True, stop=True),
                                      reads=["wg", "xm"], writes=[pk])
                                bname = "lbr" if g == 0 else "lbi"
                                fw.op("act", lambda e, dst=dst, bname=bname: e.activation(dst[:, c0:c0 + N], ps[:, 0:N], AF.Sigmoid, bias=SM(bname, d * 4 + ch)),
                                      reads=[pk, "sm"], writes=["rr" if g == 0 else "ii"])
                        fw.op("act", lambda e: e.activation(rr[:], rr[:], AF.Exp, scale=cc[:, d * 4 + ch:d * 4 + ch + 1]), reads=["rr", "cc"], writes=["rr"])
                        fw.op("dve", lambda e: e.tensor_tensor(ii[:], ii[:], xm[:], ALU.mult), reads=["ii", "xm"], writes=["ii"])
                        fw.op("dve", lambda e: e.tensor_tensor(bbv[:], rr[:], rr[:], ALU.mult), reads=["rr"], writes=["bbv"])
                        fw.op("dve", lambda e: e.tensor_scalar(bbv[:], bbv[:], -1.0, 1.0, ALU.mult, ALU.add), reads=["bbv"], writes=["bbv"])
                        fw.op("act", lambda e: e.activation(bbv[:], bbv[:], AF.Sqrt), reads=["bbv"], writes=["bbv"])
                        fw.op("dve", lambda e: e.tensor_tensor(bbv[:], bbv[:], ii[:], ALU.mult), reads=["bbv", "ii"], writes=["bbv"])
                        hh = hf[d]
                        hk = "hf%d" % d
                        if d == 0:
                            fw.op("dve", lambda e: e.tensor_tensor_scan(hh[:], rr[:], bbv[:], 0.0, ALU.mult, ALU.add), reads=["rr", "bbv"], writes=[hk])
                        else:
                            fw.op("dve", lambda e: e.tensor_tensor_scan(hh[:, 0:NCTX][:, ::-1], rr[:, 0:NCTX][:, ::-1],
                                                                        bbv[:, 0:NCTX][:, ::-1], 0.0, ALU.mult, ALU.add),
                                  reads=["rr", "bbv"], writes=[hk])
                            fw.op("dve", lambda e: e.tensor_tensor_scan(hh[:, NCTX:T][:, ::-1], rr[:, NCTX:T][:, ::-1], bbv[:, NCTX:T][:, ::-1],
                                                                        hh[:, 0:1], ALU.mult, ALU.add),
                                  reads=["rr", "bbv", hk], writes=[hk])
                    fw.op("dve", lambda e: e.tensor_tensor(hf[0][:], hf[0][:], hf[1][:], ALU.add), reads=["hf0", "hf1"], writes=["hf0"])
                    fw.op("act", lambda e: e.activation(ii[:], lg[:], AF.Square), reads=["lg", "ii"], writes=["ii"])
                    fw.op("dve", lambda e: e.tensor_scalar(ii[:], ii[:], 0.044715, 1.0, ALU.mult, ALU.add), reads=["ii"], writes=["ii"])
                    fw.op("dve", lambda e: e.tensor_tensor(ii[:], ii[:], lg[:], ALU.mult), reads=["ii", "lg"], writes=["ii"])
                    fw.op("act", lambda e: e.activation(ii[:], ii[:], AF.Sigmoid, scale=1.5957691216057308), reads=["ii"], writes=["ii"])
                    fw.op("dve", lambda e: e.tensor_tensor(ii[:], ii[:], lg[:], ALU.mult), reads=["ii", "lg"], writes=["ii"])
                    fw.op("dve", lambda e: e.tensor_tensor(lro[:], ii[:], hf[0][:], ALU.mult), reads=["ii", "hf0"], writes=["lro"])
                    fw.dma("sp", lrT[:, ch, :], lro[:], reads=["lro"], writes=[("lrT", ch)])
                fw.barrier()
            if stop_after == "F":
                break

            with ExitStack() as ph:
                wpa = sbt(ph, "wpa", [64, 8, D], BF16)
                wpc = sbt(ph, "wpc", [128, 4, D], BF16)
                wpl = sbt(ph, "wpl", [128, 4, D], BF16)
                wo = sbt(ph, "wo", [128, 8, D], BF16)
                oin = Rot([sbt(ph, "oin%d" % i, [64, 8, 512], BF16) for i in range(2)], "oin")
                cin = Rot([sbt(ph, "cin%d" % i, [128, 4, 512], BF16) for i in range(2)], "cin")
                lin = Rot([sbt(ph, "lin%d" % i, [128, 4, 512], BF16) for i in range(2)], "lin")
                gin = Rot([sbt(ph, "gin%d" % i, [128, 3, 512], F32) for i in range(2)], "gin")
                mt = Rot([sbt(ph, "mt%d" % i, [128, 3, 512], F32) for i in range(2)], "mt")
                mm = Rot([sbt(ph, "mm%d" % i, [128, 8, 512], BF16) for i in range(2)], "mm")
                zp = Rot([sbt(ph, "zg%d" % i, [128, 8, 512], F32) for i in range(1)], "zg")
                hp = Rot([sbt(ph, "hg%d" % i, [128, 8, 512], F32) for i in range(1)], "hg")
                pools = {"sq": Rot([sbt(ph, "sqg%d" % i, [128, 8, 512], F32) for i in range(1)], "sqg"),
                         "st": Rot([sbt(ph, "stg%d" % i, [128, 3, 512], F32) for i in range(1)], "stg"),
                         "ps": Rot([pst(ph, "psg%d" % i, [128, 512]) for i in range(2)], "psg")}
                pp = Rot([pst(ph, "pg%d" % i, [128, 512]) for i in range(6)], "pg")
                fw.dma("pool", wpa[:], w_pa[l].rearrange("(h p) n -> p h n", p=64), writes=["wpa"])
                fw.dma("pool", wpc[:], w_pc[l].rearrange("(kc p) n -> p kc n", p=128), writes=["wpc"])
                fw.dma("pool", wpl[:], w_pl[l].rearrange("(kc p) n -> p kc n", p=128), writes=["wpl"])
                fw.dma("pool", wo[:], w_o[l].rearrange("(kc p) n -> p kc n", p=128), writes=["wo"])
                for ti, (c0, N) in enumerate(TILES):
                    ctx = (ti == 0)
                    if ctx and l == L - 1:
                        continue
                    oi, oik = oin.next()
                    ci, cik = cin.next()
                    li, lik = lin.next()
                    fw.dma("sp", oi[:, :, 0:N], oT[:, :, c0:c0 + N].rearrange("h p n -> p h n"), writes=[oik])
                    fw.dma("sp", ci[:, :, 0:N], cbT[:, :, c0:c0 + N], writes=[cik])
                    fw.dma("sp", li[:, :, 0:N], lrT[:, :, c0:c0 + N], writes=[lik])
                    hh, hk = hp.next()
                    fw.dma("sp", hh[:, :, 0:N], hT[:, :, c0:c0 + N], reads=[("hT", ti)], writes=[hk])
                    m_, mk_ = mm.next()
                    for i in range(8):
                        gi, gik = gin.next()
                        fw.dma("sp", gi[:, :, 0:N], glT[:, :, c0:c0 + N].rearrange("p (b i) n -> p b i n", b=3)[:, :, i, :], writes=[gik])
                        fw.op("act", lambda e: e.activation(gi[:, :, 0:N], gi[:, :, 0:N], AF.Sigmoid), reads=[gik], writes=[gik])
                        pa, pak = pp.next()
                        for h in range(8):
                            fw.op("pe", lambda e, h=h: e.matmul(pa[:, 0:N], wpa[:, h, i * 128:(i + 1) * 128], oi[:, h, 0:N], start=(h == 0), stop=(h == 7)),
                                  reads=["wpa", oik], writes=[pak], inc=(h == 7))
                        pb_, pbk = pp.next()
                        for kc in range(4):
                            fw.op("pe", lambda e, kc=kc: e.matmul(pb_[:, 0:N], wpc[:, kc, i * 128:(i + 1) * 128], ci[:, kc, 0:N], start=(kc == 0), stop=(kc == 3)),
                                  reads=["wpc", cik], writes=[pbk], inc=(kc == 3))
                        pc_, pck = pp.next()
                        for kc in range(4):
                            fw.op("pe", lambda e, kc=kc: e.matmul(pc_[:, 0:N], wpl[:, kc, i * 128:(i + 1) * 128], li[:, kc, 0:N], start=(kc == 0), stop=(kc == 3)),
                                  reads=["wpl", lik], writes=[pck], inc=(kc == 3))
                        t3, t3k = mt.next()
                        fw.op("dve", lambda e: e.tensor_tensor(t3[:, 0, 0:N], pa[:, 0:N], gi[:, 0, 0:N], ALU.mult), reads=[pak, gik], writes=[t3k])
                        fw.op("dve", lambda e: e.tensor_tensor(t3[:, 1, 0:N], pb_[:, 0:N], gi[:, 1, 0:N], ALU.mult), reads=[pbk, gik], writes=[t3k])
                        fw.op("dve", lambda e: e.tensor_tensor(t3[:, 2, 0:N], pc_[:, 0:N], gi[:, 2, 0:N], ALU.mult), reads=[pck, gik], writes=[t3k])
                        fw.op("dve", lambda e: e.tensor_tensor(t3[:, 0, 0:N], t3[:, 0, 0:N], t3[:, 1, 0:N], ALU.add), reads=[t3k], writes=[t3k])
                        fw.op("dve", lambda e: e.tensor_tensor(m_[:, i, 0:N], t3[:, 0, 0:N], t3[:, 2, 0:N], ALU.add), reads=[t3k], writes=[mk_])
                    z, zk = zp.next()
                    for io in range(8):
                        po, pok = pp.next()
                        for i in range(8):
                            fw.op("pe", lambda e, i=i: e.matmul(po[:, 0:N], wo[:, i, io * 128:(io + 1) * 128], m_[:, i, 0:N], start=(i == 0), stop=(i == 7)),
                                  reads=["wo", mk_], writes=[pok], inc=(i == 7))
                        fw.op("act", lambda e, io=io: e.activation(z[:, io, 0:N], po[:, 0:N], AF.Identity, bias=SM("b_o", io)), reads=[pok, "sm"], writes=[zk])
                        fw.op("dve", lambda e, io=io: e.tensor_scalar(z[:, io, 0:N], z[:, io, 0:N], MOD(2, io, ctx), None, ALU.mult), reads=[zk, "modT"], writes=[zk])
                        fw.op("dve", lambda e, io=io: e.scalar_tensor_tensor(z[:, io, 0:N], hh[:, io, 0:N], ALPHA, z[:, io, 0:N], ALU.mult, ALU.add),
                              reads=[zk, hk], writes=[zk])
                    st, stk = ln_stats(pools, z, zk, N)
                    ln_apply(st, stk, z, zk, N, hh, hk, lambda kc: SM("ln1g", kc), lambda kc: SM("ln1b", kc))
                    fw.dma("sp", hT[:, :, c0:c0 + N], hh[:, :, 0:N], reads=[hk], writes=[("hT", ti)])
                fw.barrier()
            if stop_after == "G":
                break

            with ExitStack() as ph:
                zp = Rot([sbt(ph, "zh%d" % i, [128, 8, 512], F32) for i in range(2)], "zh")
                up = Rot([sbt(ph, "uh%d" % i, [128, 8, 512], F32) for i in range(2)], "uh")
                ub = Rot([sbt(ph, "ubh%d" % i, [128, 8, 512], BF16) for i in range(2)], "ubh")
                rw = sbt(ph, "rw", [128, 8, NE], F32)
                lgt = Rot([sbt(ph, "lgt%d" % i, [128, NE], F32) for i in range(3)], "lgt")
                m8 = Rot([sbt(ph, "m8%d" % i, [128, 12], F32) for i in range(3)], "m8")
                pools = {"sq": Rot([sbt(ph, "sqh%d" % i, [128, 8, 512], F32) for i in range(1)], "sqh"),
                         "st": Rot([sbt(ph, "sth%d" % i, [128, 3, 512], F32) for i in range(2)], "sth"),
                         "ps": Rot([pst(ph, "psh%d" % i, [128, 512]) for i in range(2)], "psh")}
                pl = Rot([pst(ph, "pl%d" % i, [128, NE]) for i in range(2)], "pl")
                ptr = Rot([pst(ph, "ptr%d" % i, [NE, 128]) for i in range(2)], "ptr")
                fw.dma("sp", rw[:], router_w[l].rearrange("(kc p) e -> p kc e", p=128), writes=["rw"])
                for ti, (c0, N) in enumerate(TILES):
                    ctx = (ti == 0)
                    if ctx and l == L - 1:
                        continue
                    z, zk = zp.next()
                    fw.dma("sp", z[:, :, 0:N], hT[:, :, c0:c0 + N], writes=[zk])
                    st, stk = ln_stats(pools, z, zk, N)
                    u, uk = up.next()
                    ln_apply(st, stk, z, zk, N, u, uk, lambda kc: MOD(4, kc, ctx), lambda kc: MOD(3, kc, ctx))
                    ubt, ubk = ub.next()
                    for kc in range(8):
                        fw.op("dve", lambda e, kc=kc: e.tensor_copy(ubt[:, kc, 0:N], u[:, kc, 0:N]), reads=[uk], writes=[ubk])
                    fw.dma("sp", u2T[:, :, c0:c0 + N], ubt[:, :, 0:N], reads=[ubk], writes=[("u2T", ti)])
                    for s in range(N // 128):
                        plg, plk = pl.next()
                        for kc in range(8):
                            fw.op("pe", lambda e, kc=kc: e.matmul(plg[:, :], u[:, kc, s * 128:(s + 1) * 128], rw[:, kc, :], start=(kc == 0), stop=(kc == 7)),
                                  reads=[uk, "rw"], writes=[plk], inc=(kc == 7))
                        lt, ltk = lgt.next()
                        mt8, m8k = m8.next()
                        fw.op("dve", lambda e: e.tensor_tensor(lt[:], plg[:, :], SM("rb"), ALU.add), reads=[plk, "sm"], writes=[ltk])
                        fw.op("dve", lambda e: e.max(out=mt8[:, 0:8], in_=lt[:]), reads=[ltk], writes=[m8k])
                        fw.op("dve", lambda e: e.tensor_scalar(mt8[:, 8:9], mt8[:, 0:1], -1.0, None, ALU.mult), reads=[m8k], writes=[m8k])
                        mk2, mk2k = lgt.next()
                        fw.op("dve", lambda e: e.tensor_scalar(mk2[:], lt[:], mt8[:, 3:4], None, ALU.is_ge), reads=[ltk, m8k], writes=[mk2k])
                        fw.op("act", lambda e: e.activation(lt[:], lt[:], AF.Exp, bias=mt8[:, 8:9]), reads=[ltk, m8k], writes=[ltk])
                        fw.op("dve", lambda e: e.tensor_tensor(lt[:], lt[:], mk2[:], ALU.mult), reads=[ltk, mk2k], writes=[ltk])
                        fw.op("dve", lambda e: e.reduce_sum(mt8[:, 9:10], lt[:], AX.X), reads=[ltk, m8k], writes=[m8k])
                        fw.op("dve", lambda e: e.reciprocal(mt8[:, 10:11], mt8[:, 9:10]), reads=[m8k], writes=[m8k])
                        fw.op("dve", lambda e: e.tensor_scalar(lt[:], lt[:], mt8[:, 10:11], None, ALU.mult), reads=[ltk, m8k], writes=[ltk])
                        pt_, ptk_ = ptr.next()
                        fw.op("pe", lambda e: e.matmul(pt_[:, :], lt[:], ident_f[:], start=True, stop=True), reads=[ltk, "ident_f"], writes=[ptk_])
                        fw.op("act", lambda e: e.activation(pT[:, c0 + s * 128:c0 + (s + 1) * 128], pt_[:, :], AF.Identity), reads=[ptk_], writes=["pT"])
                fw.dma("sp", pT_d, pT[:], reads=["pT"], writes=["pT_d"])
                fw.barrier()
            if stop_after == "H":
                break

            last_layer = (l == L - 1)

            def _tiles(n):
                out_, a_ = [], 0
                while a_ < n:
                    out_.append((a_, min(512, n - a_)))
                    a_ += 512
                return out_

            if last_layer:
                GSM = 1408
                GROUPS = [(NCTX, _tiles(1408), 1408), (NCTX + 1408, _tiles(1408), 1408), (NCTX + 2816, _tiles(1280), 1280)]
            else:
                GSM = 1088
                GROUPS = [(g * 1088, _tiles(1088), 1088) for g in range(4)]
            with ExitStack() as ph:
                u2 = sbt(ph, "u2", [128, 8, GSM], BF16)
                yacc = sbt(ph, "yacc", [128, 8, GSM], F32)
                hid = sbt(ph, "hid", [128, 8, GSM], BF16)
                pbc = sbt(ph, "pbc", [128, GSM], F32)
                pm = sbt(ph, "pm", [NE, GSM], F32)
                bdn = sbt(ph, "bdn", [NE, D], F32)
                bu1 = sbt(ph, "bu1", [128, NE * 8], F32)
                bg7 = sbt(ph, "bg7", [128, NE * 8], F32)
                NWB = 4 if last_layer else 5
                wbuf = [sbt(ph, "wb%d" % i, [128, 8, 1024], BF16) for i in range(NWB)]
                tr = Rot([sbt(ph, "tr%d" % i, [128, 3, 512], F32) for i in range(2)], "tr")
                SW = 64 if last_layer else 128
                hgl = sbt(ph, "hgl", [128, 8, SW], F32)
                pools = {"sq": Rot([sbt(ph, "sqi%d" % i, [128, 8, SW], F32) for i in range(1)], "sqi"),
                         "st": Rot([sbt(ph, "sti%d" % i, [128, 3, SW], F32) for i in range(1)], "sti"),
                         "ps": Rot([pst(ph, "psi%d" % i, [128, 512]) for i in range(2)], "psi")}
                pp = Rot([pst(ph, "pi%d" % i, [128, 512]) for i in range(6)], "pi")
                fw.dma("sp", bdn[:], b_dn[l], writes=["bdn"])
                bgu_v = SM("bgu").rearrange("p (e t j) -> p e t j", e=NE, t=2)
                fw.op("dve", lambda e: e.tensor_scalar(bg7[:].rearrange("p (e j) -> p e j", e=NE), bgu_v[:, :, 0, :], -1.0, 7.0, ALU.mult, ALU.add),
                      reads=["sm"], writes=["bg7"])
                fw.op("dve", lambda e: e.tensor_scalar(bu1[:].rearrange("p (e j) -> p e j", e=NE), bgu_v[:, :, 1, :], 1.0, None, ALU.add),
                      reads=["sm"], writes=["bu1"])
                pieces = [(gi_, ex, k) for gi_ in range(len(GROUPS)) for ex in range(NE) for k in range(3)]
                emitted = [0]

                def emit_piece(pi):
                    if pi >= len(pieces):
                        return
                    _, ex, k = pieces[pi]
                    wb = wbuf[pi % NWB]
                    wk = ("wb", pi % NWB)
                    if k < 2:
                        wv = w_gu[l][ex].rearrange("(kc p) n -> p kc n", p=128)
                        srcs = [k * 512, k * 512 + 256, D + k * 512, D + k * 512 + 256]
                    else:
                        wv = w_dn[l][ex].rearrange("(kc p) n -> p kc n", p=128)
                        srcs = [0, 256, 512, 768]
                    for q in range(2):
                        c = srcs[2 * q]
                        fw.dma("pool", wb[:, :, q * 512:(q + 1) * 512], wv[:, :, c:c + 512], writes=[wk])

                def need(pi):
                    while emitted[0] <= min(pi + NWB - 2, len(pieces) - 1):
                        emit_piece(emitted[0])
                        emitted[0] += 1

                pi = 0
                for gi_, (g0, gt, GS) in enumerate(GROUPS):
                    fw.dma("act", u2[:, :, 0:GS], u2T[:, :, g0:g0 + GS], reads=["yacc"], writes=["u2"])
                    for (t0, N) in gt:
                        for io in range(8):
                            ps, pk = pp.next()
                            fw.op("pe", lambda e: e.matmul(ps[:, 0:N], bdn[:, io * 128:(io + 1) * 128], pT[:, g0 + t0:g0 + t0 + N], start=True, stop=True),
                                  reads=["bdn", "pT"], writes=[pk])
                            fw.op("act", lambda e: e.activation(yacc[:, io, t0:t0 + N], ps[:, 0:N], AF.Identity), reads=[pk], writes=["yacc"])
                    for ex in range(NE):
                        fw.op("dve", lambda e: e.tensor_scalar(pm[:, 0:GS], pT[:, g0:g0 + GS], ident_f[0:NE, ex:ex + 1], None, ALU.mult),
                              reads=["pT", "ident_f", "pm"], writes=["pm"])
                        for (t0, N) in gt:
                            ps, pk = pp.next()
                            fw.op("pe", lambda e: e.matmul(ps[:, 0:N], ones_f[0:NE, :], pm[:, t0:t0 + N], start=True, stop=True),
                                  reads=["ones_f", "pm"], writes=[pk])
                            fw.op("act", lambda e: e.activation(pbc[:, t0:t0 + N], ps[:, 0:N], AF.Identity, scale=float(D) / 1.702), reads=[pk, "hid"], writes=["pbc"])
                        for k in range(2):
                            need(pi)
                            wb, wk = wbuf[pi % NWB], ("wb", pi % NWB)
                            pi += 1
                            for jj in range(4):
                                j = k * 4 + jj
                                for (t0, N) in gt:
                                    pg_, pgk = pp.next()
                                    for kc in range(8):
                                        fw.op("pe", lambda e, kc=kc: e.matmul(pg_[:, 0:N], wb[:, kc, jj * 128:(jj + 1) * 128], u2[:, kc, t0:t0 + N], start=(kc == 0), stop=(kc == 7)),
                                              reads=[wk, "u2"], writes=[pgk], inc=(kc == 7))
                                    pu_, puk = pp.next()
                                    for kc in range(8):
                                        fw.op("pe", lambda e, kc=kc: e.matmul(pu_[:, 0:N], wb[:, kc, 512 + jj * 128:512 + (jj + 1) * 128], u2[:, kc, t0:t0 + N], start=(kc == 0), stop=(kc == 7)),
                                              reads=[wk, "u2"], writes=[puk], inc=(kc == 7))
                                    t3, t3k = tr.next()
                                    bi = ex * 8 + j
                                    fw.op("act", lambda e: e.activation(t3[:, 0, 0:N], pg_[:, 0:N], AF.Relu, bias=bg7[:, bi:bi + 1], scale=-1.0), reads=[pgk, "bg7"], writes=[t3k])
                                    fw.op("act", lambda e: e.activation(t3[:, 0, 0:N], t3[:, 0, 0:N], AF.Silu, bias=cbias[:, 0:1], scale=-1.702), reads=[t3k, "cbias"], writes=[t3k])
                                    fw.op("dve", lambda e: e.tensor_scalar(t3[:, 1, 0:N], pu_[:, 0:N], bu1[:, bi:bi + 1], 8.0, ALU.add, ALU.min), reads=[puk, "bu1"], writes=[t3k])
                                    fw.op("dve", lambda e: e.tensor_tensor(t3[:, 2, 0:N], t3[:, 0, 0:N], pbc[:, t0:t0 + N], ALU.mult), reads=[t3k, "pbc"], writes=[t3k])
                                    fw.op("dve", lambda e: e.scalar_tensor_tensor(hid[:, j, t0:t0 + N], t3[:, 1, 0:N], -6.0, t3[:, 2, 0:N], ALU.max, ALU.mult), reads=[t3k], writes=["hid"])
                        need(pi)
                        wb, wk = wbuf[pi % NWB], ("wb", pi % NWB)
                        pi += 1
                        for io in range(8):
                            for (t0, N) in gt:
                                pd, pdk = pp.next()
                                for j in range(8):
                                    fw.op("pe", lambda e, j=j: e.matmul(pd[:, 0:N], wb[:, j, io * 128:(io + 1) * 128], hid[:, j, t0:t0 + N], start=(j == 0), stop=(j == 7)),
                                          reads=[wk, "hid"], writes=[pdk], inc=(j == 7))
                                fw.op("dve", lambda e: e.tensor_tensor(yacc[:, io, t0:t0 + N], yacc[:, io, t0:t0 + N], pd[:, 0:N], ALU.add), reads=[pdk, "yacc"], writes=["yacc"])
                    subt = []
                    cc0 = g0
                    while cc0 < g0 + GS:
                        nn = min(SW - (cc0 % SW), g0 + GS - cc0)
                        subt.append((cc0, nn))
                        cc0 += nn
                    for (c0, N) in subt:
                        ctx = c0 < NCTX
                        a = c0 - g0
                        fw.dma("act", hgl[:, :, 0:N], hT[:, :, c0:c0 + N], reads=[("hTi", c0)], writes=["hgl"])
                        for io in range(8):
                            fw.op("dve", lambda e, io=io: e.tensor_scalar(yacc[:, io, a:a + N], yacc[:, io, a:a + N], MOD(5, io, ctx), None, ALU.mult),
                                  reads=["yacc", "modT"], writes=["yacc"])
                            fw.op("dve", lambda e, io=io: e.scalar_tensor_tensor(hgl[:, io, 0:N], hgl[:, io, 0:N], ALPHA, yacc[:, io, a:a + N], ALU.mult, ALU.add),
                                  reads=["yacc", "hgl"], writes=["hgl"])
                        st, stk = ln_stats(pools, hgl, "hgl", N)
                        ln_apply(st, stk, hgl, "hgl", N, hgl, "hgl", lambda kc: SM("ln2g", kc), lambda kc: SM("ln2b", kc), engs=("dve",))
                        fw.dma("act", hT[:, :, c0:c0 + N], hgl[:, :, 0:N], reads=["hgl"], writes=[("hTi", c0)])
                fw.barrier()
            if stop_after == "I" + str(l):
                break

        fw.barrier()
        fw.dma("sp", out, hT[:, :, NCTX:T], writes=["out"])
        fw.barrier()
        print("instructions:", fw.ninst, "cnt:", fw.cnt)
    return nc


def _pcol(v, p=128):
    v = np.asarray(v, np.float32)
    return np.ascontiguousarray(v.reshape(-1, p).T)


def _constants():
    ident = np.eye(128, dtype=np.float32)
    ones = np.full((128, 128), 1.0 / D, np.float32)
    rot = np.zeros((64, 64), np.float32)
    for base in (0, 32):
        for m in range(16):
            rot[base + m + 16, base + m] = -1.0
            rot[base + m, base + m + 16] = 1.0
    t = np.arange(SEQ)
    row, col = (t // GW).astype(np.float32), (t % GW).astype(np.float32)
    inv = (10000.0 ** (-np.arange(16, dtype=np.float32) / 16)).astype(np.float32)
    ang = np.zeros((64, SEQ), np.float32)
    for d in range(64):
        pos = row if d < 32 else col
        ang[d] = pos * inv[d % 16]
    cos, sin = np.cos(ang).astype(np.float32), np.sin(ang).astype(np.float32)
    sel = np.zeros((NE, NE, 128), np.float32)
    for e in range(NE):
        sel[e, e, :] = 1.0
    return ident, ones, rot, cos, sin, sel


def _attn_index():
    idx_dr = np.zeros((5, 128, 640), np.int64)
    idx_dc = np.zeros((5, 128, 640), np.int64)
    valid = np.zeros((5, 128, 640), bool)
    reps = {0: 5, 1: 0, 2: 1, 3: 30, 4: 31}
    for pat, j in reps.items():
        bs = min(max(2 * j - 4, 0), 54)
        for par in range(2):
            r = 2 * j + par
            r0 = min(max(r - 4, 0), 56)
            for c in range(64):
                cs = min(max(c - 8, 0), 48)
                q = par * 64 + c
                for i in range(10):
                    br = bs + i
                    if not (r0 <= br < r0 + 8):
                        continue
                    dr = br - r + 7
                    for kc in range(cs, cs + 16):
                        idx_dr[pat, q, i * 64 + kc] = dr
                        idx_dc[pat, q, i * 64 + kc] = kc - c + 15
                        valid[pat, q, i * 64 + kc] = True
    return idx_dr, idx_dc, valid


_CACHE = {}


def _prep_shared(inp):
    f = lambda k: np.ascontiguousarray(np.asarray(inp[k], np.float32))
    sh = {}
    sh["w_mod"] = f("w_mod")
    sh["w_in"] = f("w_in")
    sh["w_pa"] = f("w_proj_attn")
    sh["w_pc"] = f("w_proj_conv")
    sh["w_pl"] = f("w_proj_lru")
    sh["w_o"] = f("w_o")
    sh["router_w"] = f("router_w")
    sh["w_gu"] = f("exp_w_gu")
    sh["w_dn"] = f("exp_w_dn")
    sh["b_dn"] = f("exp_b_dn")
    wri = np.zeros((L, 2, 2, 4, 128, 128), np.float32)
    for g, key in enumerate(("lru_w_r", "lru_w_i")):
        w = f(key)
        for ch in range(4):
            for half in range(2):
                wri[:, g, :, ch, half * 64:(half + 1) * 64, half * 64:(half + 1) * 64] = w[:, :, ch * 2 + half]
    sh["wri"] = wri
    small = np.zeros((L, 128, NS), np.float32)

    def put(name, arr):
        o, w = _off[name]
        assert arr.shape == (128, w), (name, arr.shape)
        small[l, :, o:o + w] = arr

    for l in range(L):
        b_in = f("b_in")[l]
        put("b_mod", _pcol(f("b_mod")[l]))
        put("b_in", _pcol(b_in))
        bqk = np.zeros((128, 16), np.float32)
        bqk[0:64] = _pcol(b_in[0:1024], 64)
        put("b_qk", bqk)
        bv = np.zeros((128, 8), np.float32)
        bv[0:64] = _pcol(b_in[1024:1536], 64)
        put("b_v", bv)
        put("scw", np.stack([_pcol(f("sc_conv_w")[l, k]) for k in range(3)], -1).reshape(128, 12))
        put("lcw", np.stack([_pcol(f("lru_conv_w")[l, k]) for k in range(4)], -1).reshape(128, 16))
        put("lcb", _pcol(f("lru_conv_b")[l]))
        put("lam", np.concatenate([_pcol(f("lru_lambda")[l, d]) for d in range(2)], 1))
        put("lbr", np.concatenate([_pcol(f("lru_b_r")[l, d]) for d in range(2)], 1))
        put("lbi", np.concatenate([_pcol(f("lru_b_i")[l, d]) for d in range(2)], 1))
        put("b_o", _pcol(f("b_o")[l]))
        put("ln1g", _pcol(f("ln1_g")[l]))
        put("ln1b", _pcol(f("ln1_b")[l]))
        put("ln2g", _pcol(f("ln2_g")[l]))
        put("ln2b", _pcol(f("ln2_b")[l]))
        put("rb", np.broadcast_to(f("router_b")[l][None, :], (128, NE)).copy())
        put("bgu", np.concatenate([_pcol(f("exp_b_gu")[l, e]) for e in range(NE)], 1))
    sh["small"] = small
    idx_dr, idx_dc, valid = _attn_index()
    rpb = f("na_rpb")
    rx = rpb[:, :, idx_dr, idx_dc] * valid[None, None]
    sh["rpbx"] = np.ascontiguousarray(rx.transpose(0, 1, 3, 2, 4)).astype(np.float32)
    sh["amask"] = np.ascontiguousarray(np.where(valid, 0.0, NEG).astype(np.float32).transpose(1, 0, 2))
    ident, ones, rot, cos, sin, sel = _constants()
    sh.update(c_ident=ident, c_ones=ones, c_rot=rot, c_cos=cos, c_sin=sin)
    return sh


def _prep_core(inp, b):
    x = np.asarray(inp["x"], np.float32)[b]
    ctx = np.asarray(inp["ctx"], np.float32)[b]
    tok = np.concatenate([ctx, x], 0)
    xT = np.ascontiguousarray(tok.T.reshape(8, 128, T).transpose(1, 0, 2))
    cv = np.stack([np.asarray(inp["c"], np.float32)[b], np.asarray(inp["c_ctx"], np.float32)], -1)
    cvec = np.ascontiguousarray(cv.reshape(8, 128, 2).transpose(1, 0, 2))
    return {"xT": xT, "cvec": cvec}


def kernel(**inputs):
    if "nc" not in _CACHE:
        _CACHE["nc"] = build_program()
    nc = _CACHE["nc"]
    sh = _prep_shared(inputs)
    in_maps = []
    for b in range(NCORES):
        m = dict(sh)
        m.update(_prep_core(inputs, b))
        in_maps.append(m)
    res = run_bass_kernel_spmd(nc, in_maps, core_ids=list(range(NCORES)))
    outs = []
    for b in range(NCORES):
        o = np.asarray(res.results[b]["out"], np.float32)
        outs.append(o.transpose(2, 1, 0).reshape(SEQ, D))
    return np.stack(outs, 0).astype(np.float32)
```

```python
import numpy as np
from contextlib import ExitStack
import concourse.bass as bass
import concourse.mybir as mybir
from concourse.bass_utils import run_bass_kernel_spmd

F32 = mybir.dt.float32
BF16 = mybir.dt.bfloat16
ALU = mybir.AluOpType
AF = mybir.ActivationFunctionType
AX = mybir.AxisListType

L = 2
D = 1024
NCTX = 256
SEQ = 4096
T = NCTX + SEQ
GW = 64
NH = 8
NE = 32
PT_TOT = 7168
ALPHA = (2 * L) ** 0.25
EPS = 1e-5
NEG = -30000.0
TILES = [(0, 256)] + [(256 + 512 * i, 512) for i in range(8)]
NCORES = 4

_off = {}
_n = 0
for _name, _w in [("b_mod", 48), ("b_in", 56), ("b_qk", 16), ("b_v", 8), ("scw", 12), ("lcw", 16), ("lcb", 4),
                  ("lam", 8), ("lbr", 8), ("lbi", 8), ("b_o", 8), ("ln1g", 8), ("ln1b", 8), ("ln2g", 8),
                  ("ln2b", 8), ("rb", 32), ("bgu", 512)]:
    _off[_name] = (_n, _w)
    _n += _w
NS = _n


class FW:
    def __init__(self, nc, es, n_dma_sems=32):
        self.nc = nc
        self.eng = {"pe": nc.tensor, "dve": nc.vector, "act": nc.scalar, "pool": nc.gpsimd, "sp": nc.sync}
        self.sem = {k: es.enter_context(nc.semaphore("s_" + k)) for k in self.eng}
        self.cnt = {k: 0 for k in self.eng}
        self.seen = {k: {} for k in self.eng}
        self.dsem = [es.enter_context(nc.semaphore("d%d" % i)) for i in range(n_dma_sems)]
        self.dcnt = [0] * n_dma_sems
        self.dnext = 0
        self.dnext_sw = 0
        self.lastw = {}
        self.readers = {}
        self.ninst = 0

    def _wait(self, e, tok):
        sem, val = tok
        key = id(sem)
        if self.seen[e].get(key, 0) >= val:
            return
        self.seen[e][key] = val
        self.eng[e].wait_ge(sem, val)

    def _deps(self, e, reads, writes):
        toks = []
        for b in reads:
            if b in self.lastw:
                toks.append(self.lastw[b])
        for b in writes:
            if b in self.lastw:
                toks.append(self.lastw[b])
            toks.extend(self.readers.get(b, ()))
        for t in toks:
            if e == "pe" and t[0] is self.sem["pe"]:
                continue
            self._wait(e, t)

    def _commit(self, tok, reads, writes):
        for b in reads:
            r = self.readers.setdefault(b, [])
            if len(r) > 12:
                best = {}
                for s, v in r:
                    if id(s) not in best or best[id(s)][1] < v:
                        best[id(s)] = (s, v)
                r[:] = list(best.values())
            r.append(tok)
        for b in writes:
            self.lastw[b] = tok
            self.readers[b] = []

    def op(self, e, fn, reads=(), writes=(), inc=True):
        self._deps(e, reads, writes)
        ins = fn(self.eng[e])
        tok = (self.sem[e], self.cnt[e] + 1)
        if inc:
            ins.then_inc(self.sem[e], 1)
            self.cnt[e] += 1
        self._commit(tok, reads, writes)
        self.ninst += 1
        return ins

    def dma(self, e, out, in_, reads=(), writes=(), **kw):
        half = len(self.dsem) // 2
        if e == "pool":
            k = half + self.dnext_sw
            self.dnext_sw = (self.dnext_sw + 1) % (len(self.dsem) - half)
        else:
            k = self.dnext
            self.dnext = (self.dnext + 1) % half
        if self.dcnt[k] > 0:
            self._wait(e, (self.dsem[k], self.dcnt[k]))
        self._deps(e, reads, writes)
        ins = self.eng[e].dma_start(out=out, in_=in_, **kw)
        self.dcnt[k] += 16
        ins.then_inc(self.dsem[k], 16)
        tok = (self.dsem[k], self.dcnt[k])
        self._commit(tok, reads, writes)
        self.ninst += 1
        return tok

    def barrier(self):
        for e in self.eng:
            for k in range(len(self.dsem)):
                if self.dcnt[k]:
                    self._wait(e, (self.dsem[k], self.dcnt[k]))
            for k in self.eng:
                if k != e and self.cnt[k]:
                    self._wait(e, (self.sem[k], self.cnt[k]))
        self.lastw = {}
        self.readers = {}


class Rot:
    def __init__(self, tiles, name):
        self.tiles = tiles
        self.name = name
        self.i = 0

    def next(self):
        k = self.i % len(self.tiles)
        self.i += 1
        return self.tiles[k], (self.name, k)


def build_program(stop_after=None, dbg=()):
    nc = bass.Bass("TRN2", target_bir_lowering=False)
    dbg = set(dbg)

    def din(name, shape, dt=F32):
        return nc.dram_tensor(name, list(shape), dt, kind="ExternalInput").ap()

    def dscr(name, shape, dt=F32):
        kind = "ExternalOutput" if name in dbg else "Internal"
        return nc.dram_tensor(name, list(shape), dt, kind=kind).ap()

    xT = din("xT", [128, 8, T])
    cvec = din("cvec", [128, 8, 2])
    w_mod = din("w_mod", [L, D, 6 * D])
    w_in = din("w_in", [L, D, PT_TOT])
    w_pa = din("w_pa", [L, 512, D])
    w_pc = din("w_pc", [L, 512, D])
    w_pl = din("w_pl", [L, 512, D])
    w_o = din("w_o", [L, D, D])
    wri = din("wri", [L, 2, 2, 4, 128, 128])
    router_w = din("router_w", [L, D, NE])
    w_gu = din("w_gu", [L, NE, D, 2 * D])
    w_dn = din("w_dn", [L, NE, D, D])
    b_dn = din("b_dn", [L, NE, D])
    small = din("small", [L, 128, NS])
    rpbx = din("rpbx", [L, NH, 128, 5, 640])
    amask = din("amask", [128, 5, 640])
    c_ident = din("c_ident", [128, 128])
    c_ones = din("c_ones", [128, 128])
    c_rot = din("c_rot", [64, 64])
    c_cos = din("c_cos", [64, SEQ])
    c_sin = din("c_sin", [64, SEQ])
    out = nc.dram_tensor("out", [128, 8, SEQ], F32, kind="ExternalOutput").ap()

    hT = dscr("hT", [128, 8, T])
    qT = dscr("qT", [NH, 64, T], BF16)
    kT = dscr("kT", [NH, 64, T], BF16)
    vtok = dscr("vtok", [128, 34, 512], BF16)
    secT = dscr("secT", [5, 128, 4, T])
    glT = dscr("glT", [128, 24, T])
    oT = dscr("oT", [NH, 64, T], BF16)
    cbT = dscr("cbT", [128, 4, T], BF16)
    lrT = dscr("lrT", [128, 4, T], BF16)
    u2T = dscr("u2T", [128, 8, T], BF16)
    pT_d = dscr("pT_d", [NE, T])
    yT = dscr("yT", [128, 8, T])

    with ExitStack() as es:
        fw = FW(nc, es)

        uid = [0]

        def sbt(st, name, shape, dt):
            uid[0] += 1
            return st.enter_context(nc.sbuf_tensor("%s_%d" % (name, uid[0]), list(shape), dt))

        def pst(st, name, shape, dt=F32):
            uid[0] += 1
            return st.enter_context(nc.psum_tensor("%s_%d" % (name, uid[0]), list(shape), dt))

        ident_f = sbt(es, "ident_f", [128, 128], F32)
        ident_b = sbt(es, "ident_b", [128, 128], BF16)
        ones_f = sbt(es, "ones_f", [128, 128], F32)
        rot_b = sbt(es, "rot_b", [64, 64], BF16)
        sm = sbt(es, "sm", [128, NS], F32)
        modT = sbt(es, "modT", [128, 48, 2], F32)
        pT = sbt(es, "pT", [NE, T], F32)
        cbias = sbt(es, "cbias", [128, 1], F32)
        fw.op("dve", lambda e: e.memset(cbias[:], 1.702 * 7.0), writes=["cbias"])
        fw.dma("sp", ident_f[:], c_ident, writes=["ident_f"])
        fw.dma("sp", ones_f[:], c_ones, writes=["ones_f"])
        fw.dma("pool", ident_b[:], c_ident, writes=["ident_b"])
        fw.dma("pool", rot_b[:], c_rot, writes=["rot_b"])

        def SM(name, i=None, n=1, rows=128):
            o, w = _off[name]
            if i is None:
                return sm[0:rows, o:o + w]
            return sm[0:rows, o + i:o + i + n]

        def ln_stats(st_pools, z, zkey, N):
            sq, sqk = st_pools["sq"].next()
            for kc in range(8):
                fw.op("act", lambda e, kc=kc: e.activation(sq[:, kc, 0:N], z[:, kc, 0:N], AF.Square),
                      reads=[zkey], writes=[sqk])
            pm, pmk = st_pools["ps"].next()
            pq, pqk = st_pools["ps"].next()
            for kc in range(8):
                fw.op("pe", lambda e, kc=kc: e.matmul(pm[:, 0:N], ones_f[:], z[:, kc, 0:N], start=(kc == 0), stop=(kc == 7)),
                      reads=[zkey, "ones_f"], writes=[pmk], inc=(kc == 7))
            for kc in range(8):
                fw.op("pe", lambda e, kc=kc: e.matmul(pq[:, 0:N], ones_f[:], sq[:, kc, 0:N], start=(kc == 0), stop=(kc == 7)),
                      reads=[sqk, "ones_f"], writes=[pqk], inc=(kc == 7))
            st, stk = st_pools["st"].next()
            fw.op("act", lambda e: e.activation(st[:, 0, 0:N], pm[:, 0:N], AF.Identity), reads=[pmk], writes=[stk])
            fw.op("dve", lambda e: e.tensor_tensor(st[:, 2, 0:N], st[:, 0, 0:N], st[:, 0, 0:N], ALU.mult), reads=[stk], writes=[stk])
            fw.op("dve", lambda e: e.tensor_tensor(st[:, 1, 0:N], pq[:, 0:N], st[:, 2, 0:N], ALU.subtract), reads=[stk, pqk], writes=[stk])
            fw.op("dve", lambda e: e.tensor_scalar(st[:, 1, 0:N], st[:, 1, 0:N], EPS, None, ALU.add), reads=[stk], writes=[stk])
            fw.op("act", lambda e: e.activation(st[:, 1, 0:N], st[:, 1, 0:N], AF.Sqrt), reads=[stk], writes=[stk])
            fw.op("dve", lambda e: e.reciprocal(st[:, 1, 0:N], st[:, 1, 0:N]), reads=[stk], writes=[stk])
            fw.op("dve", lambda e: e.tensor_tensor(st[:, 2, 0:N], st[:, 0, 0:N], st[:, 1, 0:N], ALU.mult), reads=[stk], writes=[stk])
            return st, stk

        def ln_apply(st, stk, z, zkey, N, outt, outkey, gain_fn, bias_fn, engs=("dve",)):
            for kc in range(8):
                e1 = engs[kc % len(engs)]
                fw.op(e1, lambda e, kc=kc: e.tensor_tensor(z[:, kc, 0:N], z[:, kc, 0:N], st[:, 1, 0:N], ALU.mult),
                      reads=[zkey, stk], writes=[zkey])
                fw.op(e1, lambda e, kc=kc: e.tensor_tensor(z[:, kc, 0:N], z[:, kc, 0:N], st[:, 2, 0:N], ALU.subtract),
                      reads=[zkey, stk], writes=[zkey])
                fw.op("act", lambda e, kc=kc: e.activation(outt[:, kc, 0:N], z[:, kc, 0:N], AF.Identity,
                                                           bias=bias_fn(kc), scale=gain_fn(kc)),
                      reads=[zkey, "modT", "sm"], writes=[outkey])


        for l in range(L):
            fw.dma("sp", sm[:], small[l], writes=["sm"])
            with ExitStack() as ph:
                csb = sbt(ph, "csb", [128, 8, 2], F32)
                cs2 = sbt(ph, "cs2", [128, 8, 2], F32)
                wm = Rot([sbt(ph, "wm%d" % i, [128, 8, 512], F32) for i in range(2)], "wm")
                pmod = pst(ph, "pmod", [128, 48, 2])
                fw.dma("sp", csb[:], cvec, writes=["csb"])
                fw.op("act", lambda e: e.activation(cs2[:], csb[:], AF.Silu), reads=["csb"], writes=["cs2"])
                for og in range(12):
                    wt, wk = wm.next()
                    fw.dma("sp", wt[:], w_mod[l][:, og * 512:(og + 1) * 512].rearrange("(kc p) n -> p kc n", p=128), writes=[wk])
                    for oc in range(4):
                        for kc in range(8):
                            fw.op("pe", lambda e, oc=oc, kc=kc: e.matmul(pmod[:, og * 4 + oc, :], wt[:, kc, oc * 128:(oc + 1) * 128],
                                                                         cs2[:, kc, :], start=(kc == 0), stop=(kc == 7)),
                                  reads=[wk, "cs2"], writes=["pmod"], inc=(kc == 7 and oc == 3))
                for j in range(2):
                    fw.op("dve", lambda e, j=j: e.tensor_tensor(modT[:, :, j], pmod[:, :, j], SM("b_mod"), ALU.add),
                          reads=["pmod", "sm"], writes=["modT"])
                for base in (8, 32):
                    fw.op("dve", lambda e, base=base: e.tensor_scalar_add(modT[:, base:base + 8, :], modT[:, base:base + 8, :], 1.0),
                          reads=["modT"], writes=["modT"])
                fw.barrier()
            if stop_after == "A":
                break

            def MOD(which, kc, ctx):
                j = 1 if ctx else 0
                return modT[:, which * 8 + kc, j:j + 1]

            with ExitStack() as ph:
                uT_sb = sbt(ph, "uT_sb", [128, 8, T], BF16)
                with ExitStack() as pb:
                    zp = Rot([sbt(pb, "zb%d" % i, [128, 8, 512], F32) for i in range(2)], "zb")
                    pools = {"sq": Rot([sbt(pb, "sqb%d" % i, [128, 8, 512], F32) for i in range(1)], "sqb"),
                             "st": Rot([sbt(pb, "stb%d" % i, [128, 3, 512], F32) for i in range(2)], "stb"),
                             "ps": Rot([pst(pb, "psb%d" % i, [128, 512]) for i in range(4)], "psb")}
                    for ti, (c0, N) in enumerate(TILES):
                        z, zk = zp.next()
                        fw.dma("sp", z[:, :, 0:N], (xT if l == 0 else hT)[:, :, c0:c0 + N], writes=[zk])
                        st, stk = ln_stats(pools, z, zk, N)
                        ctx = (ti == 0)
                        ln_apply(st, stk, z, zk, N, uT_sb[:, :, c0:c0 + N], ("uT", ti),
                                 lambda kc: MOD(1, kc, ctx), lambda kc: MOD(0, kc, ctx), engs=("dve",))
                    fw.barrier()
                if stop_after == "B":
                    if "uT_dbg" in dbg:
                        pass
                    break
                with ExitStack() as pc:
                    wq = Rot([sbt(pc, "wq%d" % i, [128, 8, 512], BF16) for i in range(2)], "wq")
                    ev = Rot([sbt(pc, "ev%d" % i, [128, 4, 512], F32) for i in range(2)], "ev")
                    evb = Rot([sbt(pc, "evb%d" % i, [64, 512], BF16) for i in range(3)], "evb")
                    rt = Rot([sbt(pc, "rt%d" % i, [64, 2, 512], F32) for i in range(2)], "rt")
                    ob = Rot([sbt(pc, "ob%d" % i, [64, 512], BF16) for i in range(3)], "ob")
                    cos_t = sbt(pc, "cos_t", [64, SEQ], F32)
                    sin_t = sbt(pc, "sin_t", [64, SEQ], F32)
                    bq8 = sbt(pc, "bq8", [64, 8], F32)
                    vsb = sbt(pc, "vsb", [128, 34, 512], BF16)
                    pp = Rot([pst(pc, "pc%d" % i, [128, 512]) for i in range(6)], "pc")
                    fw.dma("sp", cos_t[:], c_cos, writes=["cos"])
                    fw.dma("sp", sin_t[:], c_sin, writes=["sin"])
                    fw.op("dve", lambda e: e.tensor_scalar(bq8[:], SM("b_qk", 0, 8, rows=64), 0.125, None, ALU.mult), reads=["sm"], writes=["bq8"])
                    uall = [("uT", ti) for ti in range(len(TILES))]
                    for sec in range(2):
                        wt, wk = wq.next()
                        fw.dma("pool", wt[:], w_in[l][:, sec * 512:(sec + 1) * 512].rearrange("(kc p) n -> p kc n", p=128), writes=[wk])
                        dst = qT if sec == 0 else kT
                        for h in range(NH):
                            for ti, (c0, N) in enumerate(TILES):
                                ps, pk = pp.next()
                                for kc in range(8):
                                    fw.op("pe", lambda e, kc=kc: e.matmul(ps[0:64, 0:N], wt[:, kc, h * 64:(h + 1) * 64], uT_sb[:, kc, c0:c0 + N],
                                                                          start=(kc == 0), stop=(kc == 7)),
                                          reads=[wk, ("uT", ti)], writes=[pk], inc=(kc == 7))
                                eb, ek = evb.next()
                                if sec == 0:
                                    fw.op("act", lambda e: e.activation(eb[:, 0:N], ps[0:64, 0:N], AF.Identity, bias=bq8[:, h:h + 1], scale=0.125),
                                          reads=[pk, "bq8"], writes=[ek])
                                else:
                                    fw.op("act", lambda e: e.activation(eb[:, 0:N], ps[0:64, 0:N], AF.Identity, bias=SM("b_qk", 8 + h, 1, rows=64)),
                                          reads=[pk, "sm"], writes=[ek])
                                if ti == 0:
                                    fw.dma("sp", dst[h][:, c0:c0 + N], eb[:, 0:N], reads=[ek], writes=[("qk", sec, h, ti)])
                                    continue
                                t0 = c0 - NCTX
                                pr, prk = pp.next()
                                fw.op("pe", lambda e: e.matmul(pr[0:64, 0:N], rot_b[:], eb[:, 0:N], start=True, stop=True),
                                      reads=[ek, "rot_b"], writes=[prk])
                                r2, rk = rt.next()
                                fw.op("dve", lambda e: e.tensor_tensor(r2[:, 0, 0:N], eb[:, 0:N], cos_t[:, t0:t0 + N], ALU.mult),
                                      reads=[ek, "cos"], writes=[rk])
                                fw.op("dve", lambda e: e.tensor_tensor(r2[:, 1, 0:N], pr[0:64, 0:N], sin_t[:, t0:t0 + N], ALU.mult),
                                      reads=[prk, "sin"], writes=[rk])
                                o2, ok = ob.next()
                                fw.op("dve", lambda e: e.tensor_tensor(o2[:, 0:N], r2[:, 0, 0:N], r2[:, 1, 0:N], ALU.add),
                                      reads=[rk], writes=[ok])
                                fw.dma("sp", dst[h][:, c0:c0 + N], o2[:, 0:N], reads=[ok], writes=[("qk", sec, h, ti)])
                    wt, wk = wq.next()
                    fw.dma("pool", wt[:], w_in[l][:, 1024:1536].rearrange("(kc p) n -> p kc n", p=128), writes=[wk])
                    for tc in range(34):
                        ps, pk = pp.next()
                        for kc in range(8):
                            fw.op("pe", lambda e, kc=kc: e.matmul(ps[:, :], uT_sb[:, kc, tc * 128:(tc + 1) * 128], wt[:, kc, :],
                                                                  start=(kc == 0), stop=(kc == 7)),
                                  reads=[wk] + uall, writes=[pk], inc=(kc == 7))
                        fw.op("act" if tc % 2 else "dve",
                              (lambda e: e.activation(vsb[:, tc, :], ps[:, :], AF.Identity)) if tc % 2 else (lambda e: e.tensor_copy(vsb[:, tc, :], ps[:, :])),
                              reads=[pk], writes=["vsb"])
                    fw.dma("sp", vtok, vsb[:], reads=["vsb"], writes=["vtok"])
                    for g in range(11):
                        col0 = 1536 + g * 512
                        wt, wk = wq.next()
                        fw.dma("pool", wt[:], w_in[l][:, col0:col0 + 512].rearrange("(kc p) n -> p kc n", p=128), writes=[wk])
                        for ti, (c0, N) in enumerate(TILES):
                            et, ek = ev.next()
                            for oc in range(4):
                                ps, pk = pp.next()
                                for kc in range(8):
                                    fw.op("pe", lambda e, kc=kc, oc=oc: e.matmul(ps[:, 0:N], wt[:, kc, oc * 128:(oc + 1) * 128], uT_sb[:, kc, c0:c0 + N],
                                                                                 start=(kc == 0), stop=(kc == 7)),
                                          reads=[wk, ("uT", ti)], writes=[pk], inc=(kc == 7))
                                bch = (col0 // 128) + oc
                                fw.op("act" if oc % 2 else "dve",
                                      (lambda e, oc=oc, bch=bch: e.activation(et[:, oc, 0:N], ps[:, 0:N], AF.Identity, bias=SM("b_in", bch))) if oc % 2 else
                                      (lambda e, oc=oc, bch=bch: e.tensor_scalar(et[:, oc, 0:N], ps[:, 0:N], SM("b_in", bch), None, ALU.add)),
                                      reads=[pk, "sm"], writes=[ek])
                            if g < 5:
                                fw.dma("sp", secT[g][:, :, c0:c0 + N], et[:, :, 0:N], reads=[ek], writes=[("sec", g, ti)])
                            else:
                                fw.dma("sp", glT[:, (g - 5) * 4:(g - 4) * 4, c0:c0 + N], et[:, :, 0:N], reads=[ek], writes=[("gl", g, ti)])
                    fw.barrier()
            if stop_after == "C":
                break

            with ExitStack() as ph:
                qh = Rot([sbt(ph, "qh%d" % i, [64, T], BF16) for i in range(2)], "qh")
                kh = Rot([sbt(ph, "kh%d" % i, [64, T], BF16) for i in range(2)], "kh")
                vh = Rot([sbt(ph, "vh%d" % i, [128, 34, 64], BF16) for i in range(2)], "vh")
                bf = Rot([sbt(ph, "bf%d" % i, [128, 5, 640], F32) for i in range(1)], "bf")
                mk = sbt(ph, "mk", [128, 5, 640], F32)
                bb = Rot([sbt(ph, "bb%d" % i, [128, 5, 640], BF16) for i in range(2)], "bb")
                oh = Rot([sbt(ph, "oh%d" % i, [64, T], BF16) for i in range(2)], "oh")
                pe_ = Rot([sbt(ph, "pe%d" % i, [128, 896], BF16) for i in range(4)], "pex")
                pn = Rot([sbt(ph, "pn%d" % i, [128, 896], BF16) for i in range(4)], "pn")
                pts = Rot([sbt(ph, "pts%d" % i, [128, 7, 128], BF16) for i in range(4)], "pts")
                stt = Rot([sbt(ph, "stt%d" % i, [128, 4], F32) for i in range(8)], "stt")
                pS = Rot([pst(ph, "pS%d" % i, [128, 1024]) for i in range(2)], "pS")
                pT_ = Rot([pst(ph, "pT%d" % i, [128, 7, 128], BF16) for i in range(2)], "pTT")
                pO = Rot([pst(ph, "pO%d" % i, [64, 128]) for i in range(2)], "pO")
                fw.dma("sp", mk[:], amask, writes=["mk"])

                heads = {}

                def load_head(h):
                    q_, qk_ = qh.next()
                    k_, kk_ = kh.next()
                    v_, vk_ = vh.next()
                    f_, fk_ = bf.next()
                    b_, bk_ = bb.next()
                    o_, ok_ = oh.next()
                    fw.dma("sp", q_[:], qT[h], writes=[qk_])
                    fw.dma("sp", k_[:], kT[h], writes=[kk_])
                    fw.dma("sp", v_[:], vtok[:, :, h * 64:(h + 1) * 64], writes=[vk_])
                    fw.dma("sp", f_[:], rpbx[l][h], writes=[fk_])
                    fw.op("dve", lambda e: e.tensor_tensor(b_[:], f_[:], mk[:], ALU.add), reads=[fk_, "mk"], writes=[bk_])
                    heads[h] = dict(q=q_, qk=qk_, k=k_, kk=kk_, v=v_, vk=vk_, b=b_, bk=bk_, o=o_, ok=ok_)

                its = []
                for h in range(NH):
                    for j in range(32):
                        bs = min(max(2 * j - 4, 0), 54)
                        its.append(dict(h=h, kind="na", qc0=NCTX + 128 * j, kc0=NCTX + 64 * bs, pat={0: 1, 1: 2, 30: 3, 31: 4}.get(j, 0),
                                        ncols=896, vch=[2 + bs // 2 + c for c in range(5)] + [0, 1], last=False))
                    for qc in range(2):
                        its.append(dict(h=h, kind="ctx", qc0=qc * 128, ncols=256, vch=[0, 1], last=(qc == 1)))

                def st0(it):
                    H = heads[it["h"]]
                    q_, k_, b_ = H["q"], H["k"], H["b"]
                    S, Sk = pS.next()
                    it["S"], it["Sk"] = S, Sk
                    qc0 = it["qc0"]
                    if it["kind"] == "na":
                        kc0, pat = it["kc0"], it["pat"]
                        fw.op("pe", lambda e: e.matmul(S[:, 0:512], q_[:, qc0:qc0 + 128], k_[:, kc0:kc0 + 512], start=True, stop=False),
                              reads=[H["qk"], H["kk"]], writes=[Sk], inc=False)
                        fw.op("pe", lambda e: e.matmul(S[:, 0:512], ident_b[:], b_[:, pat, 0:512], start=False, stop=True),
                              reads=[H["bk"], "ident_b"], writes=[Sk], inc=False)
                        fw.op("pe", lambda e: e.matmul(S[:, 512:640], q_[:, qc0:qc0 + 128], k_[:, kc0 + 512:kc0 + 640], start=True, stop=False),
                              reads=[H["qk"], H["kk"]], writes=[Sk], inc=False)
                        fw.op("pe", lambda e: e.matmul(S[:, 512:640], ident_b[:], b_[:, pat, 512:640], start=False, stop=True),
                              reads=[H["bk"], "ident_b"], writes=[Sk], inc=False)
                        fw.op("pe", lambda e: e.matmul(S[:, 640:896], q_[:, qc0:qc0 + 128], k_[:, 0:256], start=True, stop=True),
                              reads=[H["qk"], H["kk"]], writes=[Sk])
                    else:
                        fw.op("pe", lambda e: e.matmul(S[:, 0:256], q_[:, qc0:qc0 + 128], k_[:, 0:256], start=True, stop=True),
                              reads=[H["qk"], H["kk"]], writes=[Sk])

                def st1(it):
                    S, Sk, ncols = it["S"], it["Sk"], it["ncols"]
                    sst, ssk = stt.next()
                    fw.op("dve", lambda e: e.reduce_max(sst[:, 0:1], S[:, 0:ncols], AX.X), reads=[Sk], writes=[ssk])
                    fw.op("dve", lambda e: e.tensor_scalar(sst[:, 1:2], sst[:, 0:1], -1.0, None, ALU.mult), reads=[ssk], writes=[ssk])
                    px, pxk = pe_.next()
                    fw.op("act", lambda e: e.activation(px[:, 0:ncols], S[:, 0:ncols], AF.Exp, bias=sst[:, 1:2], accum_out=sst[:, 2:3]),
                          reads=[Sk, ssk], writes=[pxk, ssk])
                    it["sst"], it["ssk"], it["px"], it["pxk"] = sst, ssk, px, pxk

                def st1b(it):
                    sst, ssk, px, pxk, ncols = it["sst"], it["ssk"], it["px"], it["pxk"], it["ncols"]
                    fw.op("dve", lambda e: e.reciprocal(sst[:, 3:4], sst[:, 2:3]), reads=[ssk], writes=[ssk])
                    pnt, pnk = pn.next()
                    fw.op("dve", lambda e: e.tensor_scalar(pnt[:, 0:ncols], px[:, 0:ncols], sst[:, 3:4], None, ALU.mult),
                          reads=[pxk, ssk], writes=[pnk])
                    it["pn"], it["pnk"] = pnt, pnk

                def st2(it):
                    pnt, pnk, nch = it["pn"], it["pnk"], it["ncols"] // 128
                    ptp, ptk = pT_.next()
                    for c in range(nch):
                        fw.op("pe", lambda e, c=c: e.transpose(ptp[:, c, :], pnt[:, c * 128:(c + 1) * 128], ident_b[:]),
                              reads=[pnk, "ident_b"], writes=[ptk], inc=(c == nch - 1))
                    ptt, pttk = pts.next()
                    fw.op("act", lambda e: e.activation(ptt[:, 0:nch, :], ptp[:, 0:nch, :], AF.Identity), reads=[ptk], writes=[pttk])
                    it["pt"], it["ptk"] = ptt, pttk

                def st3(it):
                    H = heads[it["h"]]
                    v_, o_, h = H["v"], H["o"], it["h"]
                    ptt, pttk, nch, vch, qc0 = it["pt"], it["ptk"], it["ncols"] // 128, it["vch"], it["qc0"]
                    po, pok = pO.next()
                    for c in range(nch):
                        fw.op("pe", lambda e, c=c: e.matmul(po[:, :], v_[:, vch[c], :], ptt[:, c, :], start=(c == 0), stop=(c == nch - 1)),
                              reads=[H["vk"], pttk], writes=[pok], inc=(c == nch - 1))
                    fw.op("dve", lambda e: e.tensor_scalar(o_[:, qc0:qc0 + 128], po[:, :], SM("b_v", h, 1, rows=64), None, ALU.add),
                          reads=[pok, "sm"], writes=[H["ok"]])
                    if it["last"]:
                        fw.dma("sp", oT[h], o_[:], reads=[H["ok"]], writes=[("oT", h)])

                load_head(0)
                load_head(1)
                nit = len(its)
                for s in range(nit + 4):
                    if s < nit:
                        st0(its[s])
                    if 0 <= s - 1 < nit:
                        st1(its[s - 1])
                    if 0 <= s - 2 < nit:
                        st1b(its[s - 2])
                    if 0 <= s - 3 < nit:
                        st2(its[s - 3])
                    if 0 <= s - 4 < nit:
                        st3(its[s - 4])
                        hh_ = its[s - 4]["h"]
                        if its[s - 4]["last"] and hh_ + 2 < NH:
                            load_head(hh_ + 2)
                fw.barrier()
            if stop_after == "D":
                break

            SEGS = [(0, NCTX), (NCTX, T)]
            with ExitStack() as ph:
                a3 = [Rot([sbt(ph, "cv%d_%d" % (s, i), [128, T], F32) for i in range(2)], "cv%d" % s) for s in range(3)]
                acc = Rot([sbt(ph, "cacc%d" % i, [128, T], F32) for i in range(2)], "cacc")
                cbo = Rot([sbt(ph, "cbo%d" % i, [128, T], BF16) for i in range(2)], "cbo")
                for ch in range(4):
                    tl = []
                    for s in range(3):
                        t_, k_ = a3[s].next()
                        fw.dma("sp", t_[:], secT[s][:, ch, :], writes=[k_])
                        tl.append((t_, k_))
                    (sbt_, sbk), (cg, cgk), (sx, sxk) = tl
                    ac, ack = acc.next()
                    fw.op("dve", lambda e: e.tensor_tensor(cg[:], cg[:], sx[:], ALU.mult), reads=[cgk, sxk], writes=[cgk])
                    w = lambda k: SM("scw", ch * 3 + k)
                    fw.op("dve", lambda e: e.tensor_scalar(ac[:], cg[:], w(1), None, ALU.mult), reads=[cgk, "sm"], writes=[ack])
                    for (a, b) in SEGS:
                        fw.op("dve", lambda e, a=a, b=b: e.scalar_tensor_tensor(ac[:, a + 1:b], cg[:, a:b - 1], w(0), ac[:, a + 1:b], ALU.mult, ALU.add),
                              reads=[cgk, ack, "sm"], writes=[ack])
                        fw.op("dve", lambda e, a=a, b=b: e.scalar_tensor_tensor(ac[:, a:b - 1], cg[:, a + 1:b], w(2), ac[:, a:b - 1], ALU.mult, ALU.add),
                              reads=[cgk, ack, "sm"], writes=[ack])
                    co, cok = cbo.next()
                    fw.op("dve", lambda e: e.tensor_tensor(co[:], ac[:], sbt_[:], ALU.mult), reads=[ack, sbk], writes=[cok])
                    fw.dma("sp", cbT[:, ch, :], co[:], reads=[cok], writes=[("cbT", ch)])
                fw.barrier()
            if stop_after == "E":
                break

            with ExitStack() as ph:
                lx = sbt(ph, "lx", [128, T], F32)
                xm = sbt(ph, "xm", [128, T], F32)
                lg = sbt(ph, "lg", [128, T], F32)
                rr = sbt(ph, "rr", [128, T], F32)
                ii = sbt(ph, "ii", [128, T], F32)
                bbv = sbt(ph, "bbv", [128, T], F32)
                hf = [sbt(ph, "hf%d" % d, [128, T], F32) for d in range(2)]
                lro = sbt(ph, "lro", [128, T], BF16)
                wg = sbt(ph, "wg", [128, 2, 2, 128], F32)
                cc = sbt(ph, "cc", [128, 8], F32)
                pp = Rot([pst(ph, "pf%d" % i, [128, 512]) for i in range(4)], "pf")
                fw.op("act", lambda e: e.activation(cc[:], SM("lam"), AF.Exp, scale=-1.0), reads=["sm"], writes=["cc"])
                fw.op("act", lambda e: e.activation(cc[:], cc[:], AF.Ln, bias=1.0), reads=["cc"], writes=["cc"])
                fw.op("dve", lambda e: e.tensor_scalar(cc[:], cc[:], -8.0, None, ALU.mult), reads=["cc"], writes=["cc"])
                for ch in range(4):
                    fw.dma("sp", lx[:], secT[3][:, ch, :], writes=["lx"])
                    fw.dma("sp", lg[:], secT[4][:, ch, :], writes=["lg"])
                    fw.dma("sp", wg[:], wri[l][:, :, ch].rearrange("g d k m -> k g d m"), writes=["wg"])
                    w = lambda k: SM("lcw", ch * 4 + k)
                    fw.op("dve", lambda e: e.tensor_scalar(xm[:], lx[:], w(2), SM("lcb", ch), ALU.mult, ALU.add), reads=["lx", "sm"], writes=["xm"])
                    for (a, b) in SEGS:
                        fw.op("dve", lambda e, a=a, b=b: e.scalar_tensor_tensor(xm[:, a + 2:b], lx[:, a:b - 2], w(0), xm[:, a + 2:b], ALU.mult, ALU.add),
                              reads=["lx", "xm", "sm"], writes=["xm"])
                        fw.op("dve", lambda e, a=a, b=b: e.scalar_tensor_tensor(xm[:, a + 1:b], lx[:, a:b - 1], w(1), xm[:, a + 1:b], ALU.mult, ALU.add),
                              reads=["lx", "xm", "sm"], writes=["xm"])
                        fw.op("dve", lambda e, a=a, b=b: e.scalar_tensor_tensor(xm[:, a:b - 1], lx[:, a + 1:b], w(3), xm[:, a:b - 1], ALU.mult, ALU.add),
                              reads=["lx", "xm", "sm"], writes=["xm"])
                    for d in range(2):
                        for (c0, N) in TILES:
                            for g, dst in ((0, rr), (1, ii)):
                                ps, pk = pp.next()
                                fw.op("pe", lambda e, g=g: e.matmul(ps[:, 0:N], wg[:, g, d, :], xm[:, c0:c0 + N], start=True, stop=True),
                                      reads=["wg", "xm"], writes=[pk])
                                bname = "lbr" if g == 0 else "lbi"
                                fw.op("act", lambda e, dst=dst, bname=bname: e.activation(dst[:, c0:c0 + N], ps[:, 0:N], AF.Sigmoid, bias=SM(bname, d * 4 + ch)),
                                      reads=[pk, "sm"], writes=["rr" if g == 0 else "ii"])
                        fw.op("act", lambda e: e.activation(rr[:], rr[:], AF.Exp, scale=cc[:, d * 4 + ch:d * 4 + ch + 1]), reads=["rr", "cc"], writes=["rr"])
                        fw.op("dve", lambda e: e.tensor_tensor(ii[:], ii[:], xm[:], ALU.mult), reads=["ii", "xm"], writes=["ii"])
                        fw.op("dve", lambda e: e.tensor_tensor(bbv[:], rr[:], rr[:], ALU.mult), reads=["rr"], writes=["bbv"])
                        fw.op("dve", lambda e: e.tensor_scalar(bbv[:], bbv[:], -1.0, 1.0, ALU.mult, ALU.add), reads=["bbv"], writes=["bbv"])
                        fw.op("act", lambda e: e.activation(bbv[:], bbv[:], AF.Sqrt), reads=["bbv"], writes=["bbv"])
                        fw.op("dve", lambda e: e.tensor_tensor(bbv[:], bbv[:], ii[:], ALU.mult), reads=["bbv", "ii"], writes=["bbv"])
                        hh = hf[d]
                        hk = "hf%d" % d
                        if d == 0:
                            fw.op("dve", lambda e: e.tensor_tensor_scan(hh[:], rr[:], bbv[:], 0.0, ALU.mult, ALU.add), reads=["rr", "bbv"], writes=[hk])
                        else:
                            fw.op("dve", lambda e: e.tensor_tensor_scan(hh[:, 0:NCTX][:, ::-1], rr[:, 0:NCTX][:, ::-1],
                                                                        bbv[:, 0:NCTX][:, ::-1], 0.0, ALU.mult, ALU.add),
                                  reads=["rr", "bbv"], writes=[hk])
                            fw.op("dve", lambda e: e.tensor_tensor_scan(hh[:, NCTX:T][:, ::-1], rr[:, NCTX:T][:, ::-1], bbv[:, NCTX:T][:, ::-1],
                                                                        hh[:, 0:1], ALU.mult, ALU.add),
                                  reads=["rr", "bbv", hk], writes=[hk])
                    fw.op("dve", lambda e: e.tensor_tensor(hf[0][:], hf[0][:], hf[1][:], ALU.add), reads=["hf0", "hf1"], writes=["hf0"])
                    fw.op("act", lambda e: e.activation(ii[:], lg[:], AF.Square), reads=["lg", "ii"], writes=["ii"])
                    fw.op("dve", lambda e: e.tensor_scalar(ii[:], ii[:], 0.044715, 1.0, ALU.mult, ALU.add), reads=["ii"], writes=["ii"])
                    fw.op("dve", lambda e: e.tensor_tensor(ii[:], ii[:], lg[:], ALU.mult), reads=["ii", "lg"], writes=["ii"])
                    fw.op("act", lambda e: e.activation(ii[:], ii[:], AF.Sigmoid, scale=1.5957691216057308), reads=["ii"], writes=["ii"])
                    fw.op("dve", lambda e: e.tensor_tensor(ii[:], ii[:], lg[:], ALU.mult), reads=["ii", "lg"], writes=["ii"])
                    fw.op("dve", lambda e: e.tensor_tensor(lro[:], ii[:], hf[0][:], ALU.mult), reads=["ii", "hf0"], writes=["lro"])
                    fw.dma("sp", lrT[:, ch, :], lro[:], reads=["lro"], writes=[("lrT", ch)])
                fw.barrier()
            if stop_after == "F":
                break

            with ExitStack() as ph:
                wpa = sbt(ph, "wpa", [64, 8, D], BF16)
                wpc = sbt(ph, "wpc", [128, 4, D], BF16)
                wpl = sbt(ph, "wpl", [128, 4, D], BF16)
                wo = sbt(ph, "wo", [128, 8, D], BF16)
                oin = Rot([sbt(ph, "oin%d" % i, [64, 8, 512], BF16) for i in range(2)], "oin")
                cin = Rot([sbt(ph, "cin%d" % i, [128, 4, 512], BF16) for i in range(2)], "cin")
                lin = Rot([sbt(ph, "lin%d" % i, [128, 4, 512], BF16) for i in range(2)], "lin")
                gin = Rot([sbt(ph, "gin%d" % i, [128, 3, 512], F32) for i in range(2)], "gin")
                mt = Rot([sbt(ph, "mt%d" % i, [128, 3, 512], F32) for i in range(2)], "mt")
                mm = Rot([sbt(ph, "mm%d" % i, [128, 8, 512], BF16) for i in range(2)], "mm")
                zp = Rot([sbt(ph, "zg%d" % i, [128, 8, 512], F32) for i in range(1)], "zg")
                hp = Rot([sbt(ph, "hg%d" % i, [128, 8, 512], F32) for i in range(1)], "hg")
                pools = {"sq": Rot([sbt(ph, "sqg%d" % i, [128, 8, 512], F32) for i in range(1)], "sqg"),
                         "st": Rot([sbt(ph, "stg%d" % i, [128, 3, 512], F32) for i in range(1)], "stg"),
                         "ps": Rot([pst(ph, "psg%d" % i, [128, 512]) for i in range(2)], "psg")}
                pp = Rot([pst(ph, "pg%d" % i, [128, 512]) for i in range(6)], "pg")
                fw.dma("pool", wpa[:], w_pa[l].rearrange("(h p) n -> p h n", p=64), writes=["wpa"])
                fw.dma("pool", wpc[:], w_pc[l].rearrange("(kc p) n -> p kc n", p=128), writes=["wpc"])
                fw.dma("pool", wpl[:], w_pl[l].rearrange("(kc p) n -> p kc n", p=128), writes=["wpl"])
                fw.dma("pool", wo[:], w_o[l].rearrange("(kc p) n -> p kc n", p=128), writes=["wo"])
                for ti, (c0, N) in enumerate(TILES):
                    ctx = (ti == 0)
                    if ctx and l == L - 1:
                        continue
                    oi, oik = oin.next()
                    ci, cik = cin.next()
                    li, lik = lin.next()
                    fw.dma("sp", oi[:, :, 0:N], oT[:, :, c0:c0 + N].rearrange("h p n -> p h n"), writes=[oik])
                    fw.dma("sp", ci[:, :, 0:N], cbT[:, :, c0:c0 + N], writes=[cik])
                    fw.dma("sp", li[:, :, 0:N], lrT[:, :, c0:c0 + N], writes=[lik])
                    hh, hk = hp.next()
                    fw.dma("sp", hh[:, :, 0:N], (xT if l == 0 else hT)[:, :, c0:c0 + N], reads=[("hT", ti)], writes=[hk])
                    m_, mk_ = mm.next()
                    for i in range(8):
                        gi, gik = gin.next()
                        fw.dma("sp", gi[:, :, 0:N], glT[:, :, c0:c0 + N].rearrange("p (b i) n -> p b i n", b=3)[:, :, i, :], writes=[gik])
                        fw.op("act", lambda e: e.activation(gi[:, :, 0:N], gi[:, :, 0:N], AF.Sigmoid), reads=[gik], writes=[gik])
                        pa, pak = pp.next()
                        for h in range(8):
                            fw.op("pe", lambda e, h=h: e.matmul(pa[:, 0:N], wpa[:, h, i * 128:(i + 1) * 128], oi[:, h, 0:N], start=(h == 0), stop=(h == 7)),
                                  reads=["wpa", oik], writes=[pak], inc=(h == 7))
                        pb_, pbk = pp.next()
                        for kc in range(4):
                            fw.op("pe", lambda e, kc=kc: e.matmul(pb_[:, 0:N], wpc[:, kc, i * 128:(i + 1) * 128], ci[:, kc, 0:N], start=(kc == 0), stop=(kc == 3)),
                                  reads=["wpc", cik], writes=[pbk], inc=(kc == 3))
                        pc_, pck = pp.next()
                        for kc in range(4):
                            fw.op("pe", lambda e, kc=kc: e.matmul(pc_[:, 0:N], wpl[:, kc, i * 128:(i + 1) * 128], li[:, kc, 0:N], start=(kc == 0), stop=(kc == 3)),
                                  reads=["wpl", lik], writes=[pck], inc=(kc == 3))
                        t3, t3k = mt.next()
                        fw.op("dve", lambda e: e.tensor_tensor(t3[:, 0, 0:N], pa[:, 0:N], gi[:, 0, 0:N], ALU.mult), reads=[pak, gik], writes=[t3k])
                        fw.op("dve", lambda e: e.tensor_tensor(t3[:, 1, 0:N], pb_[:, 0:N], gi[:, 1, 0:N], ALU.mult), reads=[pbk, gik], writes=[t3k])
                        fw.op("dve", lambda e: e.tensor_tensor(t3[:, 2, 0:N], pc_[:, 0:N], gi[:, 2, 0:N], ALU.mult), reads=[pck, gik], writes=[t3k])
                        fw.op("dve", lambda e: e.tensor_tensor(t3[:, 0, 0:N], t3[:, 0, 0:N], t3[:, 1, 0:N], ALU.add), reads=[t3k], writes=[t3k])
                        fw.op("dve", lambda e: e.tensor_tensor(m_[:, i, 0:N], t3[:, 0, 0:N], t3[:, 2, 0:N], ALU.add), reads=[t3k], writes=[mk_])
                    z, zk = zp.next()
                    for io in range(8):
                        po, pok = pp.next()
                        for i in range(8):
                            fw.op("pe", lambda e, i=i: e.matmul(po[:, 0:N], wo[:, i, io * 128:(io + 1) * 128], m_[:, i, 0:N], start=(i == 0), stop=(i == 7)),
                                  reads=["wo", mk_], writes=[pok], inc=(i == 7))
                        fw.op("act", lambda e, io=io: e.activation(z[:, io, 0:N], po[:, 0:N], AF.Identity, bias=SM("b_o", io)), reads=[pok, "sm"], writes=[zk])
                        fw.op("dve", lambda e, io=io: e.tensor_scalar(z[:, io, 0:N], z[:, io, 0:N], MOD(2, io, ctx), None, ALU.mult), reads=[zk, "modT"], writes=[zk])
                        fw.op("dve", lambda e, io=io: e.scalar_tensor_tensor(z[:, io, 0:N], hh[:, io, 0:N], ALPHA, z[:, io, 0:N], ALU.mult, ALU.add),
                              reads=[zk, hk], writes=[zk])
                    st, stk = ln_stats(pools, z, zk, N)
                    ln_apply(st, stk, z, zk, N, hh, hk, lambda kc: SM("ln1g", kc), lambda kc: SM("ln1b", kc))
                    fw.dma("sp", hT[:, :, c0:c0 + N], hh[:, :, 0:N], reads=[hk], writes=[("hT", ti)])
                fw.barrier()
            if stop_after == "G":
                break

            with ExitStack() as ph:
                zp = Rot([sbt(ph, "zh%d" % i, [128, 8, 512], F32) for i in range(2)], "zh")
                up = Rot([sbt(ph, "uh%d" % i, [128, 8, 512], F32) for i in range(2)], "uh")
                ub = Rot([sbt(ph, "ubh%d" % i, [128, 8, 512], BF16) for i in range(2)], "ubh")
                rw = sbt(ph, "rw", [128, 8, NE], F32)
                lgt = Rot([sbt(ph, "lgt%d" % i, [128, NE], F32) for i in range(3)], "lgt")
                m8 = Rot([sbt(ph, "m8%d" % i, [128, 12], F32) for i in range(3)], "m8")
                pools = {"sq": Rot([sbt(ph, "sqh%d" % i, [128, 8, 512], F32) for i in range(1)], "sqh"),
                         "st": Rot([sbt(ph, "sth%d" % i, [128, 3, 512], F32) for i in range(2)], "sth"),
                         "ps": Rot([pst(ph, "psh%d" % i, [128, 512]) for i in range(2)], "psh")}
                pl = Rot([pst(ph, "pl%d" % i, [128, NE]) for i in range(2)], "pl")
                ptr = Rot([pst(ph, "ptr%d" % i, [NE, 128]) for i in range(2)], "ptr")
                fw.dma("sp", rw[:], router_w[l].rearrange("(kc p) e -> p kc e", p=128), writes=["rw"])
                for ti, (c0, N) in enumerate(TILES):
                    ctx = (ti == 0)
                    if ctx and l == L - 1:
                        continue
                    z, zk = zp.next()
                    fw.dma("sp", z[:, :, 0:N], hT[:, :, c0:c0 + N], writes=[zk])
                    st, stk = ln_stats(pools, z, zk, N)
                    u, uk = up.next()
                    ln_apply(st, stk, z, zk, N, u, uk, lambda kc: MOD(4, kc, ctx), lambda kc: MOD(3, kc, ctx))
                    ubt, ubk = ub.next()
                    for kc in range(8):
                        fw.op("dve", lambda e, kc=kc: e.tensor_copy(ubt[:, kc, 0:N], u[:, kc, 0:N]), reads=[uk], writes=[ubk])
                    fw.dma("sp", u2T[:, :, c0:c0 + N], ubt[:, :, 0:N], reads=[ubk], writes=[("u2T", ti)])
                    for s in range(N // 128):
                        plg, plk = pl.next()
                        for kc in range(8):
                            fw.op("pe", lambda e, kc=kc: e.matmul(plg[:, :], u[:, kc, s * 128:(s + 1) * 128], rw[:, kc, :], start=(kc == 0), stop=(kc == 7)),
                                  reads=[uk, "rw"], writes=[plk], inc=(kc == 7))
                        lt, ltk = lgt.next()
                        mt8, m8k = m8.next()
                        fw.op("dve", lambda e: e.tensor_tensor(lt[:], plg[:, :], SM("rb"), ALU.add), reads=[plk, "sm"], writes=[ltk])
                        fw.op("dve", lambda e: e.max(out=mt8[:, 0:8], in_=lt[:]), reads=[ltk], writes=[m8k])
                        fw.op("dve", lambda e: e.tensor_scalar(mt8[:, 8:9], mt8[:, 0:1], -1.0, None, ALU.mult), reads=[m8k], writes=[m8k])
                        mk2, mk2k = lgt.next()
                        fw.op("dve", lambda e: e.tensor_scalar(mk2[:], lt[:], mt8[:, 3:4], None, ALU.is_ge), reads=[ltk, m8k], writes=[mk2k])
                        fw.op("act", lambda e: e.activation(lt[:], lt[:], AF.Exp, bias=mt8[:, 8:9]), reads=[ltk, m8k], writes=[ltk])
                        fw.op("dve", lambda e: e.tensor_tensor(lt[:], lt[:], mk2[:], ALU.mult), reads=[ltk, mk2k], writes=[ltk])
                        fw.op("dve", lambda e: e.reduce_sum(mt8[:, 9:10], lt[:], AX.X), reads=[ltk, m8k], writes=[m8k])
                        fw.op("dve", lambda e: e.reciprocal(mt8[:, 10:11], mt8[:, 9:10]), reads=[m8k], writes=[m8k])
                        fw.op("dve", lambda e: e.tensor_scalar(lt[:], lt[:], mt8[:, 10:11], None, ALU.mult), reads=[ltk, m8k], writes=[ltk])
                        pt_, ptk_ = ptr.next()
                        fw.op("pe", lambda e: e.matmul(pt_[:, :], lt[:], ident_f[:], start=True, stop=True), reads=[ltk, "ident_f"], writes=[ptk_])
                        fw.op("act", lambda e: e.activation(pT[:, c0 + s * 128:c0 + (s + 1) * 128], pt_[:, :], AF.Identity), reads=[ptk_], writes=["pT"])
                fw.dma("sp", pT_d, pT[:], reads=["pT"], writes=["pT_d"])
                fw.barrier()
            if stop_after == "H":
                break

            last_layer = (l == L - 1)

            def _tiles(n):
                out_, a_ = [], 0
                while a_ < n:
                    out_.append((a_, min(512, n - a_)))
                    a_ += 512
                return out_

            if last_layer:
                GSM = 1408
                GROUPS = [(NCTX, _tiles(1408), 1408), (NCTX + 1408, _tiles(1408), 1408), (NCTX + 2816, _tiles(1280), 1280)]
            else:
                GSM = 1088
                GROUPS = [(g * 1088, _tiles(1088), 1088) for g in range(4)]
            with ExitStack() as ph:
                u2 = sbt(ph, "u2", [128, 8, GSM], BF16)
                yacc = sbt(ph, "yacc", [128, 8, GSM], F32)
                hid = sbt(ph, "hid", [128, 8, GSM], BF16)
                pbc = sbt(ph, "pbc", [128, GSM], F32)
                pm = sbt(ph, "pm", [NE, GSM], F32)
                bdn = sbt(ph, "bdn", [NE, D], F32)
                bu1 = sbt(ph, "bu1", [128, NE * 8], F32)
                bg7 = sbt(ph, "bg7", [128, NE * 8], F32)
                NWB = 4 if last_layer else 5
                wbuf = [sbt(ph, "wb%d" % i, [128, 8, 1024], BF16) for i in range(NWB)]
                tr = Rot([sbt(ph, "tr%d" % i, [128, 3, 512], F32) for i in range(2)], "tr")
                pp = Rot([pst(ph, "pi%d" % i, [128, 512]) for i in range(6)], "pi")
                fw.dma("sp", bdn[:], b_dn[l], writes=["bdn"])
                bgu_v = SM("bgu").rearrange("p (e t j) -> p e t j", e=NE, t=2)
                fw.op("dve", lambda e: e.tensor_scalar(bg7[:].rearrange("p (e j) -> p e j", e=NE), bgu_v[:, :, 0, :], -1.0, 7.0, ALU.mult, ALU.add),
                      reads=["sm"], writes=["bg7"])
                fw.op("dve", lambda e: e.tensor_scalar(bu1[:].rearrange("p (e j) -> p e j", e=NE), bgu_v[:, :, 1, :], 1.0, None, ALU.add),
                      reads=["sm"], writes=["bu1"])
                pieces = [(gi_, ex, k) for gi_ in range(len(GROUPS)) for ex in range(NE) for k in range(3)]
                emitted = [0]

                def emit_piece(pi):
                    if pi >= len(pieces):
                        return
                    _, ex, k = pieces[pi]
                    wb = wbuf[pi % NWB]
                    wk = ("wb", pi % NWB)
                    if k < 2:
                        wv = w_gu[l][ex].rearrange("(kc p) n -> p kc n", p=128)
                        srcs = [k * 512, k * 512 + 256, D + k * 512, D + k * 512 + 256]
                    else:
                        wv = w_dn[l][ex].rearrange("(kc p) n -> p kc n", p=128)
                        srcs = [0, 256, 512, 768]
                    for q in range(2):
                        c = srcs[2 * q]
                        fw.dma("pool", wb[:, :, q * 512:(q + 1) * 512], wv[:, :, c:c + 512], writes=[wk])

                def need(pi):
                    while emitted[0] <= min(pi + NWB - 2, len(pieces) - 1):
                        emit_piece(emitted[0])
                        emitted[0] += 1

                pi = 0
                for gi_, (g0, gt, GS) in enumerate(GROUPS):
                    fw.dma("sp", u2[:, :, 0:GS], u2T[:, :, g0:g0 + GS], writes=["u2"])
                    for (t0, N) in gt:
                        for io in range(8):
                            ps, pk = pp.next()
                            fw.op("pe", lambda e: e.matmul(ps[:, 0:N], bdn[:, io * 128:(io + 1) * 128], pT[:, g0 + t0:g0 + t0 + N], start=True, stop=True),
                                  reads=["bdn", "pT"], writes=[pk])
                            fw.op("act", lambda e: e.activation(yacc[:, io, t0:t0 + N], ps[:, 0:N], AF.Identity), reads=[pk], writes=["yacc"])
                    for ex in range(NE):
                        fw.op("dve", lambda e: e.tensor_scalar(pm[:, 0:GS], pT[:, g0:g0 + GS], ident_f[0:NE, ex:ex + 1], None, ALU.mult),
                              reads=["pT", "ident_f", "pm"], writes=["pm"])
                        for (t0, N) in gt:
                            ps, pk = pp.next()
                            fw.op("pe", lambda e: e.matmul(ps[:, 0:N], ones_f[0:NE, :], pm[:, t0:t0 + N], start=True, stop=True),
                                  reads=["ones_f", "pm"], writes=[pk])
                            fw.op("act", lambda e: e.activation(pbc[:, t0:t0 + N], ps[:, 0:N], AF.Identity, scale=float(D) / 1.702), reads=[pk, "hid"], writes=["pbc"])
                        for k in range(2):
                            need(pi)
                            wb, wk = wbuf[pi % NWB], ("wb", pi % NWB)
                            pi += 1
                            for jj in range(4):
                                j = k * 4 + jj
                                for (t0, N) in gt:
                                    pg_, pgk = pp.next()
                                    for kc in range(8):
                                        fw.op("pe", lambda e, kc=kc: e.matmul(pg_[:, 0:N], wb[:, kc, jj * 128:(jj + 1) * 128], u2[:, kc, t0:t0 + N], start=(kc == 0), stop=(kc == 7)),
                                              reads=[wk, "u2"], writes=[pgk], inc=(kc == 7))
                                    pu_, puk = pp.next()
                                    for kc in range(8):
                                        fw.op("pe", lambda e, kc=kc: e.matmul(pu_[:, 0:N], wb[:, kc, 512 + jj * 128:512 + (jj + 1) * 128], u2[:, kc, t0:t0 + N], start=(kc == 0), stop=(kc == 7)),
                                              reads=[wk, "u2"], writes=[puk], inc=(kc == 7))
                                    t3, t3k = tr.next()
                                    bi = ex * 8 + j
                                    fw.op("act", lambda e: e.activation(t3[:, 0, 0:N], pg_[:, 0:N], AF.Relu, bias=bg7[:, bi:bi + 1], scale=-1.0), reads=[pgk, "bg7"], writes=[t3k])
                                    fw.op("act", lambda e: e.activation(t3[:, 0, 0:N], t3[:, 0, 0:N], AF.Silu, bias=cbias[:, 0:1], scale=-1.702), reads=[t3k, "cbias"], writes=[t3k])
                                    fw.op("dve", lambda e: e.tensor_scalar(t3[:, 1, 0:N], pu_[:, 0:N], bu1[:, bi:bi + 1], 8.0, ALU.add, ALU.min), reads=[puk, "bu1"], writes=[t3k])
                                    fw.op("dve", lambda e: e.tensor_tensor(t3[:, 2, 0:N], t3[:, 0, 0:N], pbc[:, t0:t0 + N], ALU.mult), reads=[t3k, "pbc"], writes=[t3k])
                                    fw.op("dve", lambda e: e.scalar_tensor_tensor(hid[:, j, t0:t0 + N], t3[:, 1, 0:N], -6.0, t3[:, 2, 0:N], ALU.max, ALU.mult), reads=[t3k], writes=["hid"])
                        need(pi)
                        wb, wk = wbuf[pi % NWB], ("wb", pi % NWB)
                        pi += 1
                        for io in range(8):
                            for (t0, N) in gt:
                                pd, pdk = pp.next()
                                for j in range(8):
                                    fw.op("pe", lambda e, j=j: e.matmul(pd[:, 0:N], wb[:, j, io * 128:(io + 1) * 128], hid[:, j, t0:t0 + N], start=(j == 0), stop=(j == 7)),
                                          reads=[wk, "hid"], writes=[pdk], inc=(j == 7))
                                fw.op("dve", lambda e: e.tensor_tensor(yacc[:, io, t0:t0 + N], yacc[:, io, t0:t0 + N], pd[:, 0:N], ALU.add), reads=[pdk, "yacc"], writes=["yacc"])
                    fw.dma("sp", yT[:, :, g0:g0 + GS], yacc[:, :, 0:GS], reads=["yacc"], writes=[("yT", gi_)])
                fw.barrier()
            with ExitStack() as ph:
                yp = Rot([sbt(ph, "yj%d" % i, [128, 8, 512], F32) for i in range(2)], "yj")
                hp = Rot([sbt(ph, "hj%d" % i, [128, 8, 512], F32) for i in range(2)], "hj")
                pools = {"sq": Rot([sbt(ph, "sqj%d" % i, [128, 8, 512], F32) for i in range(2)], "sqj"),
                         "st": Rot([sbt(ph, "stj%d" % i, [128, 3, 512], F32) for i in range(2)], "stj"),
                         "ps": Rot([pst(ph, "psj%d" % i, [128, 512]) for i in range(4)], "psj")}
                for ti, (c0, N) in enumerate(TILES):
                    ctx = (ti == 0)
                    if ctx and last_layer:
                        continue
                    y, yk = yp.next()
                    hh, hk = hp.next()
                    fw.dma("sp", y[:, :, 0:N], yT[:, :, c0:c0 + N], writes=[yk])
                    fw.dma("sp", hh[:, :, 0:N], hT[:, :, c0:c0 + N], writes=[hk])
                    for io in range(8):
                        fw.op("act", lambda e, io=io: e.activation(y[:, io, 0:N], y[:, io, 0:N], AF.Identity, scale=MOD(5, io, ctx)),
                              reads=[yk, "modT"], writes=[yk])
                        fw.op("dve", lambda e, io=io: e.scalar_tensor_tensor(hh[:, io, 0:N], hh[:, io, 0:N], ALPHA, y[:, io, 0:N], ALU.mult, ALU.add),
                              reads=[yk, hk], writes=[hk])
                    st, stk = ln_stats(pools, hh, hk, N)
                    ln_apply(st, stk, hh, hk, N, y, yk, lambda kc: SM("ln2g", kc), lambda kc: SM("ln2b", kc))
                    if last_layer:
                        fw.dma("sp", out[:, :, c0 - NCTX:c0 - NCTX + N], y[:, :, 0:N], reads=[yk], writes=[("out", ti)])
                    else:
                        fw.dma("sp", hT[:, :, c0:c0 + N], y[:, :, 0:N], reads=[yk], writes=[("hTj", ti)])
                fw.barrier()
            if stop_after == "I" + str(l):
                break

        fw.barrier()
        print("instructions:", fw.ninst, "cnt:", fw.cnt)
    return nc


def _pcol(v, p=128):
    v = np.asarray(v, np.float32)
    return np.ascontiguousarray(v.reshape(-1, p).T)


def _constants():
    ident = np.eye(128, dtype=np.float32)
    ones = np.full((128, 128), 1.0 / D, np.float32)
    rot = np.zeros((64, 64), np.float32)
    for base in (0, 32):
        for m in range(16):
            rot[base + m + 16, base + m] = -1.0
            rot[base + m, base + m + 16] = 1.0
    t = np.arange(SEQ)
    row, col = (t // GW).astype(np.float32), (t % GW).astype(np.float32)
    inv = (10000.0 ** (-np.arange(16, dtype=np.float32) / 16)).astype(np.float32)
    ang = np.zeros((64, SEQ), np.float32)
    for d in range(64):
        pos = row if d < 32 else col
        ang[d] = pos * inv[d % 16]
    cos, sin = np.cos(ang).astype(np.float32), np.sin(ang).astype(np.float32)
    sel = np.zeros((NE, NE, 128), np.float32)
    for e in range(NE):
        sel[e, e, :] = 1.0
    return ident, ones, rot, cos, sin, sel


def _attn_index():
    idx_dr = np.zeros((5, 128, 640), np.int64)
    idx_dc = np.zeros((5, 128, 640), np.int64)
    valid = np.zeros((5, 128, 640), bool)
    reps = {0: 5, 1: 0, 2: 1, 3: 30, 4: 31}
    for pat, j in reps.items():
        bs = min(max(2 * j - 4, 0), 54)
        for par in range(2):
            r = 2 * j + par
            r0 = min(max(r - 4, 0), 56)
            for c in range(64):
                cs = min(max(c - 8, 0), 48)
                q = par * 64 + c
                for i in range(10):
                    br = bs + i
                    if not (r0 <= br < r0 + 8):
                        continue
                    dr = br - r + 7
                    for kc in range(cs, cs + 16):
                        idx_dr[pat, q, i * 64 + kc] = dr
                        idx_dc[pat, q, i * 64 + kc] = kc - c + 15
                        valid[pat, q, i * 64 + kc] = True
    return idx_dr, idx_dc, valid


_CACHE = {}


def _prep_shared(inp):
    f = lambda k: np.ascontiguousarray(np.asarray(inp[k], np.float32))
    sh = {}
    sh["w_mod"] = f("w_mod")
    sh["w_in"] = f("w_in")
    sh["w_pa"] = f("w_proj_attn")
    sh["w_pc"] = f("w_proj_conv")
    sh["w_pl"] = f("w_proj_lru")
    sh["w_o"] = f("w_o")
    sh["router_w"] = f("router_w")
    sh["w_gu"] = f("exp_w_gu")
    sh["w_dn"] = f("exp_w_dn")
    sh["b_dn"] = f("exp_b_dn")
    wri = np.zeros((L, 2, 2, 4, 128, 128), np.float32)
    for g, key in enumerate(("lru_w_r", "lru_w_i")):
        w = f(key)
        for ch in range(4):
            for half in range(2):
                wri[:, g, :, ch, half * 64:(half + 1) * 64, half * 64:(half + 1) * 64] = w[:, :, ch * 2 + half]
    sh["wri"] = wri
    small = np.zeros((L, 128, NS), np.float32)

    def put(name, arr):
        o, w = _off[name]
        assert arr.shape == (128, w), (name, arr.shape)
        small[l, :, o:o + w] = arr

    for l in range(L):
        b_in = f("b_in")[l]
        put("b_mod", _pcol(f("b_mod")[l]))
        put("b_in", _pcol(b_in))
        bqk = np.zeros((128, 16), np.float32)
        bqk[0:64] = _pcol(b_in[0:1024], 64)
        put("b_qk", bqk)
        bv = np.zeros((128, 8), np.float32)
        bv[0:64] = _pcol(b_in[1024:1536], 64)
        put("b_v", bv)
        put("scw", np.stack([_pcol(f("sc_conv_w")[l, k]) for k in range(3)], -1).reshape(128, 12))
        put("lcw", np.stack([_pcol(f("lru_conv_w")[l, k]) for k in range(4)], -1).reshape(128, 16))
        put("lcb", _pcol(f("lru_conv_b")[l]))
        put("lam", np.concatenate([_pcol(f("lru_lambda")[l, d]) for d in range(2)], 1))
        put("lbr", np.concatenate([_pcol(f("lru_b_r")[l, d]) for d in range(2)], 1))
        put("lbi", np.concatenate([_pcol(f("lru_b_i")[l, d]) for d in range(2)], 1))
        put("b_o", _pcol(f("b_o")[l]))
        put("ln1g", _pcol(f("ln1_g")[l]))
        put("ln1b", _pcol(f("ln1_b")[l]))
        put("ln2g", _pcol(f("ln2_g")[l]))
        put("ln2b", _pcol(f("ln2_b")[l]))
        put("rb", np.broadcast_to(f("router_b")[l][None, :], (128, NE)).copy())
        put("bgu", np.concatenate([_pcol(f("exp_b_gu")[l, e]) for e in range(NE)], 1))
    sh["small"] = small
    idx_dr, idx_dc, valid = _attn_index()
    rpb = f("na_rpb")
    rx = rpb[:, :, idx_dr, idx_dc] * valid[None, None]
    sh["rpbx"] = np.ascontiguousarray(rx.transpose(0, 1, 3, 2, 4)).astype(np.float32)
    sh["amask"] = np.ascontiguousarray(np.where(valid, 0.0, NEG).astype(np.float32).transpose(1, 0, 2))
    ident, ones, rot, cos, sin, sel = _constants()
    sh.update(c_ident=ident, c_ones=ones, c_rot=rot, c_cos=cos, c_sin=sin)
    return sh


def _prep_core(inp, b):
    x = np.asarray(inp["x"], np.float32)[b]
    ctx = np.asarray(inp["ctx"], np.float32)[b]
    tok = np.concatenate([ctx, x], 0)
    xT = np.ascontiguousarray(tok.T.reshape(8, 128, T).transpose(1, 0, 2))
    cv = np.stack([np.asarray(inp["c"], np.float32)[b], np.asarray(inp["c_ctx"], np.float32)], -1)
    cvec = np.ascontiguousarray(cv.reshape(8, 128, 2).transpose(1, 0, 2))
    return {"xT": xT, "cvec": cvec}


def kernel(**inputs):
    if "nc" not in _CACHE:
        _CACHE["nc"] = build_program()
    nc = _CACHE["nc"]
    sh = _prep_shared(inputs)
    in_maps = []
    for b in range(NCORES):
        m = dict(sh)
        m.update(_prep_core(inputs, b))
        in_maps.append(m)
    res = run_bass_kernel_spmd(nc, in_maps, core_ids=list(range(NCORES)))
    outs = []
    for b in range(NCORES):
        o = np.asarray(res.results[b]["out"], np.float32)
        outs.append(o.transpose(2, 1, 0).reshape(SEQ, D))
    return np.stack(outs, 0).astype(np.float32)
```
